# Optimizing a Trainium2 kernel written in Bass

```python
import math
import jax
import jax.numpy as jnp
from jax import lax
import numpy as np


D_MODEL = 2048
BATCH = 16
SEQ = 2048
DEPTH = 2

CTX_LEN = 256
GRID_W = 64
EPS = 1e-6
N_BRANCH = 3
BRANCH_W = D_MODEL // 2

ATT_HD = 128
ATT_HEADS = BRANCH_W // ATT_HD
ATT_KV_HEADS = ATT_HEADS // 4
ATT_REP = ATT_HEADS // ATT_KV_HEADS
ATT_W = ATT_HEADS * ATT_HD
ATT_KV_W = ATT_KV_HEADS * ATT_HD
ATT_WIN = 128
ATT_BLOCK = 128
ATT_NSIDE = -(-ATT_WIN // ATT_BLOCK)
ATT_PAD = ATT_NSIDE * ATT_BLOCK
ATT_WIN_K = ATT_BLOCK + 2 * ATT_PAD
ATT_SCALE = ATT_HD ** -0.5
ROPE_BASE = 10000.0

ML_HEADS = 4
ML_HD = BRANCH_W // ML_HEADS
ML_W = ML_HEADS * ML_HD
ML_CHUNK = 64
ML_F_BIAS_LO = 3.0
ML_F_BIAS_HI = 6.0

SSM_HD = 64
SSM_HEADS = BRANCH_W // SSM_HD
SSM_GROUPS = 2
SSM_HPG = SSM_HEADS // SSM_GROUPS
SSM_STATE = 128
SSM_W = SSM_HEADS * SSM_HD
SSM_BC_W = SSM_GROUPS * SSM_STATE
SSM_CONV_CH = SSM_W + 2 * SSM_BC_W
SSM_CONV = 5
SSM_CHUNK = 64

IN_COLS = (('att_q', ATT_W), ('att_k', ATT_KV_W), ('att_v', ATT_KV_W), ('att_z', ATT_W),
           ('ml_q', ML_W), ('ml_k', ML_W), ('ml_v', ML_W), ('ml_o', ML_W), ('ml_z', ML_W),
           ('ml_gates', 4 * ML_HEADS),
           ('ssm_xbc', SSM_CONV_CH), ('ssm_dt', 2 * SSM_HEADS), ('ssm_z', SSM_W))
N_IN = 2 * ATT_W + 2 * ATT_KV_W + 5 * ML_W + 4 * ML_HEADS + SSM_CONV_CH + 2 * SSM_HEADS + SSM_W

kernel_name = 'hybrid_gated_swa_mlstm_ssd_dit'


def _rmsnorm(t, g):
    tf = t.astype(jnp.float32)
    tf = tf * lax.rsqrt(jnp.mean(tf * tf, -1, keepdims=True) + EPS)
    return (tf * g.astype(jnp.float32)).astype(t.dtype)


def _headnorm(t):
    return t * lax.rsqrt(jnp.mean(t * t, -1, keepdims=True) + EPS)


def _split_cols(u):
    out = {}
    off = 0
    for name, w in IN_COLS:
        out[name] = u[..., off:off + w]
        off += w
    return out


def _rev(t):
    return jnp.flip(t, axis=1)


def _chunks(t, ch):
    b, l = t.shape[:2]
    return jnp.moveaxis(t.reshape(b, l // ch, ch, *t.shape[2:]), 1, 0)


def _unchunks(t):
    t = jnp.moveaxis(t, 0, 1)
    return t.reshape(t.shape[0], t.shape[1] * t.shape[2], *t.shape[3:])


def _dwconv_centred(t, w, b):
    pad = w.shape[0] // 2
    y = lax.conv_general_dilated(t, w.astype(t.dtype)[:, None, :], window_strides=(1,),
                                 padding=[(pad, pad)], dimension_numbers=('NWC', 'WIO', 'NWC'),
                                 feature_group_count=t.shape[-1])
    return y + b.astype(t.dtype)


def _axial_rope_tables(row, col):
    n_freq = ATT_HD // 4
    inv = ROPE_BASE ** (-jnp.arange(n_freq, dtype=jnp.float32) / n_freq)
    ang = jnp.concatenate([row.astype(jnp.float32)[:, None] * inv,
                           col.astype(jnp.float32)[:, None] * inv], -1)
    return jnp.cos(ang), jnp.sin(ang)


def _rope(t, cos, sin):
    tf = t.astype(jnp.float32).reshape(*t.shape[:-1], ATT_HD // 2, 2)
    t0, t1 = tf[..., 0], tf[..., 1]
    cs = cos[None, :, None, :]
    sn = sin[None, :, None, :]
    out = jnp.stack([t0 * cs - t1 * sn, t0 * sn + t1 * cs], -1)
    return out.reshape(t.shape).astype(t.dtype)


def _sink_softmax(s, sink):
    m = jnp.maximum(jnp.max(s, -1, keepdims=True), sink)
    e = jnp.exp(s - m)
    return e / (jnp.sum(e, -1, keepdims=True) + jnp.exp(sink - m))


def _window_attention(q, k, v, kc, vc, sink_g):
    b_, s_ = q.shape[:2]
    nb = s_ // ATT_BLOCK
    qg = q.reshape(b_, s_, ATT_KV_HEADS, ATT_REP, ATT_HD)
    pad = ((0, 0), (ATT_PAD, ATT_PAD), (0, 0), (0, 0))
    kp = jnp.pad(k, pad)
    vp = jnp.pad(v, pad)
    sink_b = sink_g[None, :, :, None, None]

    def block(i):
        start = i * ATT_BLOCK
        qb = lax.dynamic_slice_in_dim(qg, start, ATT_BLOCK, axis=1)
        kb = lax.dynamic_slice_in_dim(kp, start, ATT_WIN_K, axis=1)
        vb = lax.dynamic_slice_in_dim(vp, start, ATT_WIN_K, axis=1)
        pos_q = start + jnp.arange(ATT_BLOCK)
        pos_k = start - ATT_PAD + jnp.arange(ATT_WIN_K)
        valid = ((jnp.abs(pos_q[:, None] - pos_k[None, :]) <= ATT_WIN)
                 & (pos_k >= 0)[None, :] & (pos_k < s_)[None, :])
        s_loc = jnp.einsum('bqgrd,bkgd->bgrqk', qb, kb).astype(jnp.float32) * ATT_SCALE
        s_loc = jnp.where(valid, s_loc, -jnp.inf)
        s_ctx = jnp.einsum('bqgrd,bkgd->bgrqk', qb, kc).astype(jnp.float32) * ATT_SCALE
        p = _sink_softmax(jnp.concatenate([s_loc, s_ctx], -1), sink_b).astype(v.dtype)
        return (jnp.einsum('bgrqk,bkgd->bqgrd', p[..., :ATT_WIN_K], vb)
                + jnp.einsum('bgrqk,bkgd->bqgrd', p[..., ATT_WIN_K:], vc))

    o = lax.map(block, jnp.arange(nb))
    return jnp.moveaxis(o, 0, 1).reshape(b_, s_, ATT_W)


def _ctx_attention(qc, kc, vc, sink_g):
    b_, l_ = qc.shape[:2]
    qg = qc.reshape(b_, l_, ATT_KV_HEADS, ATT_REP, ATT_HD)
    s = jnp.einsum('bqgrd,bkgd->bgrqk', qg, kc).astype(jnp.float32) * ATT_SCALE
    p = _sink_softmax(s, sink_g[None, :, :, None, None]).astype(vc.dtype)
    return jnp.einsum('bgrqk,bkgd->bqgrd', p, vc).reshape(b_, l_, ATT_W)


def _attention_branch(u, uc, sink, cos, sin, need_ctx):
    b_, s_ = u['att_q'].shape[:2]
    lc = uc['att_q'].shape[1]
    q = _rope(u['att_q'].reshape(b_, s_, ATT_HEADS, ATT_HD), cos, sin)
    k = _rope(u['att_k'].reshape(b_, s_, ATT_KV_HEADS, ATT_HD), cos, sin)
    v = u['att_v'].reshape(b_, s_, ATT_KV_HEADS, ATT_HD)
    kc = uc['att_k'].reshape(b_, lc, ATT_KV_HEADS, ATT_HD)
    vc = uc['att_v'].reshape(b_, lc, ATT_KV_HEADS, ATT_HD)
    sink_g = sink.astype(jnp.float32).reshape(ATT_KV_HEADS, ATT_REP)
    o = _window_attention(q, k, v, kc, vc, sink_g) * jax.nn.silu(u['att_z'])
    oc = None
    if need_ctx:
        qc = uc['att_q'].reshape(b_, lc, ATT_HEADS, ATT_HD)
        oc = _ctx_attention(qc, kc, vc, sink_g) * jax.nn.silu(uc['att_z'])
    return o, oc


def _mlstm_scan(q, k, v, ig, lf, state):
    tri = jnp.tril(jnp.ones((ML_CHUNK, ML_CHUNK), bool))

    def step(carry, inp):
        c_prev, n_prev, m_prev = carry
        qc, kc, vc, ic, fc = inp
        bcum = jnp.cumsum(fc, axis=1)
        inter = bcum + m_prev[:, None, :]
        dmat = bcum[:, :, None, :] - bcum[:, None, :, :] + ic[:, None, :, :]
        dmat = jnp.where(tri[None, :, :, None], dmat, -jnp.inf)
        mt = jnp.maximum(inter, jnp.max(dmat, axis=2))
        w = jnp.exp(dmat - mt[:, :, None, :])
        qk = jnp.einsum('bthd,bshd->btsh', qc, kc).astype(jnp.float32) * w
        e_int = jnp.exp(inter - mt)
        num = (e_int[..., None] * jnp.einsum('bthd,bhde->bthe', qc, c_prev)
               + jnp.einsum('btsh,bshe->bthe', qk, vc))
        den = e_int * jnp.einsum('bthd,bhd->bth', qc, n_prev) + jnp.sum(qk, axis=2)
        h = num / jnp.maximum(jnp.abs(den), jnp.exp(-mt))[..., None]
        m_new = mt[:, -1]
        wdec = jnp.exp(bcum[:, -1:] - bcum + ic - m_new[:, None, :])
        sp = jnp.exp(bcum[:, -1] + m_prev - m_new)
        c_new = sp[..., None, None] * c_prev + jnp.einsum('bsh,bshd,bshe->bhde', wdec, kc, vc)
        n_new = sp[..., None] * n_prev + jnp.einsum('bsh,bshd->bhd', wdec, kc)
        return (c_new, n_new, m_new), h

    xs = tuple(_chunks(t, ML_CHUNK) for t in (q, k, v, ig, lf))
    final, hs = lax.scan(step, state, xs)
    return final, _unchunks(hs)


def _mlstm_branch(u, uc, gate_bias, norm_g, need_ctx):
    gb = gate_bias.astype(jnp.float32)

    def prep(cols):
        b_, l_ = cols['ml_q'].shape[:2]
        hd = lambda t: t.reshape(b_, l_, ML_HEADS, ML_HD)
        q = hd(cols['ml_q'])
        k = hd(cols['ml_k']) * (ML_HD ** -0.5)
        v = hd(cols['ml_v'])
        g = cols['ml_gates'].astype(jnp.float32).reshape(b_, l_, 4, ML_HEADS) + gb
        return (q, k, v, g[:, :, 0], jax.nn.log_sigmoid(g[:, :, 1]),
                g[:, :, 2], jax.nn.log_sigmoid(g[:, :, 3]))

    def finish(cols, h_sum):
        b_, l_ = cols['ml_o'].shape[:2]
        o = jax.nn.sigmoid(cols['ml_o'].astype(jnp.float32)).reshape(b_, l_, ML_HEADS, ML_HD)
        hn = _headnorm(o * h_sum).reshape(b_, l_, ML_W)
        out = hn * norm_g.astype(jnp.float32) * jax.nn.silu(cols['ml_z'].astype(jnp.float32))
        return out.astype(cols['ml_z'].dtype)

    qc, kc, vc, igf_c, lff_c, igb_c, lfb_c = prep(uc)
    b_ = qc.shape[0]
    st0 = (jnp.zeros((b_, ML_HEADS, ML_HD, ML_HD), jnp.float32),
           jnp.zeros((b_, ML_HEADS, ML_HD), jnp.float32),
           jnp.zeros((b_, ML_HEADS), jnp.float32))
    st_f, hc_f = _mlstm_scan(qc, kc, vc, igf_c, lff_c, st0)
    st_b, hc_b = _mlstm_scan(_rev(qc), _rev(kc), _rev(vc), _rev(igb_c), _rev(lfb_c), st0)
    q, k, v, igf, lff, igb, lfb = prep(u)
    _, h_f = _mlstm_scan(q, k, v, igf, lff, st_f)
    _, h_b = _mlstm_scan(_rev(q), _rev(k), _rev(v), _rev(igb), _rev(lfb), st_b)
    out = finish(u, h_f + _rev(h_b))
    out_c = finish(uc, hc_f + _rev(hc_b)) if need_ctx else None
    return out, out_c


def _ssd_scan(xs, dt, a, bm, cm, h0):
    tri = jnp.tril(jnp.ones((SSM_CHUNK, SSM_CHUNK), bool))

    def step(h, inp):
        xc, dtc, bc, cc = inp
        cum = jnp.cumsum(dtc * a, axis=1)
        seg = cum[:, :, None] - cum[:, None, :]
        lmat = jnp.exp(jnp.where(tri[None, :, :, None, None], seg, -jnp.inf))
        cb = jnp.einsum('btgn,bsgn->btsg', cc, bc).astype(jnp.float32)
        mix = cb[..., None] * lmat * dtc[:, None]
        y = (jnp.einsum('btsgr,bsgrp->btgrp', mix, xc)
             + jnp.exp(cum)[..., None] * jnp.einsum('btgn,bgrnp->btgrp', cc, h))
        wdec = jnp.exp(cum[:, -1:] - cum) * dtc
        h_new = (jnp.exp(cum[:, -1])[..., None, None] * h
                 + jnp.einsum('bsgr,bsgn,bsgrp->bgrnp', wdec, bc, xc))
        return h_new, y

    seqs = tuple(_chunks(t, SSM_CHUNK) for t in (xs, dt, bm, cm))
    final, ys = lax.scan(step, h0, seqs)
    return final, _unchunks(ys)


def _ssd_branch(u, uc, conv_w, conv_b, dt_bias, a_log, d_skip, norm_g, need_ctx):
    a = (-jnp.exp(a_log.astype(jnp.float32))).reshape(2, SSM_GROUPS, SSM_HPG)
    dsk = d_skip.astype(jnp.float32).reshape(SSM_GROUPS, SSM_HPG)

    def prep(cols):
        b_, l_ = cols['ssm_xbc'].shape[:2]
        xbc = jax.nn.silu(_dwconv_centred(cols['ssm_xbc'], conv_w, conv_b))
        xs = xbc[..., :SSM_W].reshape(b_, l_, SSM_GROUPS, SSM_HPG, SSM_HD)
        bm = xbc[..., SSM_W:SSM_W + SSM_BC_W].reshape(b_, l_, SSM_GROUPS, SSM_STATE)
        cm = xbc[..., SSM_W + SSM_BC_W:].reshape(b_, l_, SSM_GROUPS, SSM_STATE)
        dt = jax.nn.softplus(cols['ssm_dt'].astype(jnp.float32).reshape(b_, l_, 2, SSM_HEADS)
                             + dt_bias.astype(jnp.float32))
        dt = dt.reshape(b_, l_, 2, SSM_GROUPS, SSM_HPG)
        return xs, bm, cm, dt[:, :, 0], dt[:, :, 1]

    def finish(cols, xs, y_sum):
        b_, l_ = xs.shape[:2]
        y = (y_sum + dsk[..., None] * xs.astype(jnp.float32)).reshape(b_, l_, SSM_W)
        y = y * jax.nn.silu(cols['ssm_z'].astype(jnp.float32))
        return _rmsnorm(y, norm_g).astype(cols['ssm_z'].dtype)

    xc, bc, cc, dtf_c, dtb_c = prep(uc)
    h0 = jnp.zeros((xc.shape[0], SSM_GROUPS, SSM_HPG, SSM_STATE, SSM_HD), jnp.float32)
    hf, yc_f = _ssd_scan(xc, dtf_c, a[0], bc, cc, h0)
    hb, yc_b = _ssd_scan(_rev(xc), _rev(dtb_c), a[1], _rev(bc), _rev(cc), h0)
    xs, bm, cm, dtf, dtb = prep(u)
    _, y_f = _ssd_scan(xs, dtf, a[0], bm, cm, hf)
    _, y_b = _ssd_scan(_rev(xs), _rev(dtb), a[1], _rev(bm), _rev(cm), hb)
    out = finish(u, xs, y_f + _rev(y_b))
    out_c = finish(uc, xc, yc_f + _rev(yc_b)) if need_ctx else None
    return out, out_c


def _merge(h, outs, w_gate, b_gate, w_branch, w_out):
    gates = jax.nn.sigmoid(h @ w_gate + b_gate).reshape(*h.shape[:-1], N_BRANCH, D_MODEL)
    proj = jnp.einsum('...nw,nwd->...nd', jnp.stack(outs, -2), w_branch)
    return jnp.sum(gates * proj, -2) @ w_out


def setup_inputs(seed: int = 0) -> dict:
    key = jax.random.key(seed)
    ks = jax.random.split(key, 24)
    f32 = jnp.float32

    def nrm(k, shape, scale):
        return jax.random.normal(k, shape, f32) * scale

    x = nrm(ks[0], (BATCH, SEQ, D_MODEL), 1.0)
    c = nrm(ks[1], (BATCH, D_MODEL), 1.0)
    ctx = nrm(ks[2], (BATCH, CTX_LEN, D_MODEL), 1.0)
    c_ctx = nrm(ks[3], (D_MODEL,), 1.0)
    w_mod = nrm(ks[4], (DEPTH, D_MODEL, 3 * D_MODEL), 0.5 * D_MODEL ** -0.5)
    b_mod = nrm(ks[5], (DEPTH, 3 * D_MODEL), 0.02)
    g_pre = 1.0 + nrm(ks[6], (DEPTH, D_MODEL), 0.02)
    w_in = nrm(ks[7], (DEPTH, D_MODEL, N_IN), D_MODEL ** -0.5)
    w_gate = nrm(ks[8], (DEPTH, D_MODEL, N_BRANCH * D_MODEL), D_MODEL ** -0.5)
    b_gate = nrm(ks[9], (DEPTH, N_BRANCH * D_MODEL), 0.02)
    att_sink = nrm(ks[10], (DEPTH, ATT_HEADS), 0.5)
    f_init = jnp.linspace(ML_F_BIAS_LO, ML_F_BIAS_HI, ML_HEADS, dtype=f32)
    zero_h = jnp.zeros((ML_HEADS,), f32)
    ml_gate_bias = (nrm(ks[11], (DEPTH, 4, ML_HEADS), 0.1)
                    + jnp.stack([zero_h, f_init, zero_h, f_init])[None])
    ml_norm = 1.0 + nrm(ks[12], (DEPTH, ML_W), 0.02)
    ssm_conv_w = nrm(ks[13], (DEPTH, SSM_CONV, SSM_CONV_CH), SSM_CONV ** -0.5)
    ssm_conv_b = nrm(ks[14], (DEPTH, SSM_CONV_CH), 0.02)
    dt0 = jnp.exp(jax.random.uniform(ks[15], (DEPTH, 2, SSM_HEADS), f32,
                                     math.log(1e-3), math.log(1e-1)))
    ssm_dt_bias = dt0 + jnp.log(-jnp.expm1(-dt0))
    ssm_a_log = jnp.log(jax.random.uniform(ks[16], (DEPTH, 2, SSM_HEADS), f32, 1.0, 16.0))
    ssm_d = 1.0 + nrm(ks[17], (DEPTH, SSM_HEADS), 0.1)
    ssm_norm = 1.0 + nrm(ks[18], (DEPTH, SSM_W), 0.02)
    w_branch = nrm(ks[19], (DEPTH, N_BRANCH, BRANCH_W, D_MODEL), BRANCH_W ** -0.5)
    w_out = nrm(ks[20], (DEPTH, D_MODEL, D_MODEL), D_MODEL ** -0.5)
    g_post = 1.0 + nrm(ks[21], (DEPTH, D_MODEL), 0.02)
    return {'x': x, 'c': c, 'ctx': ctx, 'c_ctx': c_ctx, 'w_mod': w_mod, 'b_mod': b_mod,
            'g_pre': g_pre, 'w_in': w_in, 'w_gate': w_gate, 'b_gate': b_gate,
            'att_sink': att_sink, 'ml_gate_bias': ml_gate_bias, 'ml_norm': ml_norm,
            'ssm_conv_w': ssm_conv_w, 'ssm_conv_b': ssm_conv_b, 'ssm_dt_bias': ssm_dt_bias,
            'ssm_a_log': ssm_a_log, 'ssm_d': ssm_d, 'ssm_norm': ssm_norm,
            'w_branch': w_branch, 'w_out': w_out, 'g_post': g_post}


def reference(x, c, ctx, c_ctx, w_mod, b_mod, g_pre, w_in, w_gate, b_gate, att_sink,
              ml_gate_bias, ml_norm, ssm_conv_w, ssm_conv_b, ssm_dt_bias, ssm_a_log, ssm_d,
              ssm_norm, w_branch, w_out, g_post):
    seq = x.shape[1]
    rows = seq // GRID_W
    row = jnp.repeat(jnp.arange(rows), GRID_W)
    col = jnp.tile(jnp.arange(GRID_W), rows)
    cos, sin = _axial_rope_tables(row, col)
    xc = ctx
    sc = jax.nn.silu(c)
    scc = jax.nn.silu(c_ctx)
    for l in range(DEPTH):
        need_ctx = l < DEPTH - 1
        mod = sc @ w_mod[l] + b_mod[l]
        mod_c = scc @ w_mod[l] + b_mod[l]
        shift, scale, gate = jnp.split(mod[:, None, :], 3, axis=-1)
        shift_c, scale_c, gate_c = jnp.split(mod_c, 3, axis=-1)
        h = _rmsnorm(x, g_pre[l]) * (1.0 + scale) + shift
        hc = _rmsnorm(xc, g_pre[l]) * (1.0 + scale_c) + shift_c
        u = _split_cols(h @ w_in[l])
        uc = _split_cols(hc @ w_in[l])
        o_a, o_a_c = _attention_branch(u, uc, att_sink[l], cos, sin, need_ctx)
        o_m, o_m_c = _mlstm_branch(u, uc, ml_gate_bias[l], ml_norm[l], need_ctx)
        o_s, o_s_c = _ssd_branch(u, uc, ssm_conv_w[l], ssm_conv_b[l], ssm_dt_bias[l],
                                 ssm_a_log[l], ssm_d[l], ssm_norm[l], need_ctx)
        y = _merge(h, (o_a, o_m, o_s), w_gate[l], b_gate[l], w_branch[l], w_out[l])
        x = x + gate * _rmsnorm(y, g_post[l])
        if need_ctx:
            yc = _merge(hc, (o_a_c, o_m_c, o_s_c), w_gate[l], b_gate[l], w_branch[l], w_out[l])
            xc = xc + gate_c * _rmsnorm(yc, g_post[l])
    return x
```

```python
import math
import numpy as np
import concourse.bass as bass
import concourse.mybir as mybir
from concourse.bass_utils import run_bass_kernel_spmd
from contextlib import ExitStack

F32 = mybir.dt.float32
BF16 = mybir.dt.bfloat16
AF = mybir.ActivationFunctionType
ALU = mybir.AluOpType
AX = mybir.AxisListType

D = 2048
SEQ = 2048
CTX = 256
T = SEQ + CTX
NT = T // 128
NIN = 10288
EPS = 1e-6
NCORES = 8
C_AQ, C_AK, C_AV, C_AZ = 0, 1024, 1280, 1536
C_MQ, C_MK, C_MV, C_MO, C_MZ, C_MG = 2560, 3584, 4608, 5632, 6656, 7680
C_SX, C_SDT, C_SZ = 7696, 9232, 9264


class Buf:
    __slots__ = ("name", "w", "r", "p", "wf", "excl")

    def __init__(self, name, fw=None, excl=False):
        self.name = name
        self.excl = excl
        self.w = {}
        self.r = {}
        self.p = {}
        self.wf = {}
        if fw is not None:
            fw.bufs.append(self)


class Eng:
    def __init__(self, fw, name, obj):
        self.fw = fw
        self.name = name
        self.obj = obj
        self.cnt = 0
        self.sems = []
        self.known = {}
        self.pending = []
        self.new_sem()

    def new_sem(self):
        s = self.fw.stack.enter_context(self.fw.nc.semaphore(f"s_{self.name}_{len(self.sems)}"))
        self.sems.append(s)
        self.cnt = 0


class Scope:
    def __init__(self, fw):
        self.fw = fw
        self.stack = ExitStack()

    def __enter__(self):
        return self

    def __exit__(self, *a):
        self.fw.barrier()
        self.stack.close()
        return False

    def sbuf(self, name, shape, dtype):
        self.fw.uid += 1
        return self.stack.enter_context(self.fw.nc.sbuf_tensor(f"{name}_{self.fw.uid}", list(shape), dtype))

    def psum(self, name, shape, dtype):
        self.fw.uid += 1
        return self.stack.enter_context(self.fw.nc.psum_tensor(f"{name}_{self.fw.uid}", list(shape), dtype))

    def tile(self, name, shape, dtype):
        return self.sbuf(name, shape, dtype), Buf(name, self.fw)

    def ptile(self, name, shape, dtype):
        nbytes = int(np.prod(shape[1:])) * (4 if dtype == F32 else 2)
        assert nbytes % 2048 == 0, (name, shape)
        return self.psum(name, shape, dtype), Buf(name, self.fw, excl=True)

    def ring(self, name, n, shape, dtype, psum=False):
        return Ring([(self.ptile if psum else self.tile)(f"{name}{i}", shape, dtype) for i in range(n)])


class Ring:
    def __init__(self, items):
        self.items = items
        self.i = 0

    def next(self):
        it = self.items[self.i % len(self.items)]
        self.i += 1
        return it


class FW:
    SEM_EPOCH = 30000

    def __init__(self, nc, n_dma_sems=32):
        self.nc = nc
        self.stack = ExitStack()
        self.bufs = []
        self.uid = 0
        self.pe = Eng(self, "pe", nc.tensor)
        self.act = Eng(self, "act", nc.scalar)
        self.dve = Eng(self, "dve", nc.vector)
        self.pool = Eng(self, "pool", nc.gpsimd)
        self.sp = Eng(self, "sp", nc.sync)
        self.engs = [self.pe, self.act, self.dve, self.pool, self.sp]
        self.dma_sems = [self.stack.enter_context(nc.semaphore(f"dq{i}")) for i in range(n_dma_sems)]
        self.dma_cnt = [0] * n_dma_sems
        self.n_hw = n_dma_sems - 12
        self.dma_i = 0
        self.dma_j = 0
        self.n_instr = 0
        self.n_wait = 0
        self.rr = 0
        self.cvec = self.stack.enter_context(nc.sbuf_tensor("cvec", [128, 4], F32))
        nc.gpsimd.memset(self.cvec[:, 0:1], EPS)
        nc.gpsimd.memset(self.cvec[:, 1:2], 1.0)
        nc.gpsimd.memset(self.cvec[:, 2:3], -math.log(16.0))
        nc.gpsimd.memset(self.cvec[:, 3:4], 0.0).then_inc(self.pool.sems[-1], 1)
        self.pool.cnt += 1
        for e in self.engs:
            if e is not self.pool:
                e.obj.wait_ge(self.pool.sems[-1], 1)
                e.known[self.pool.sems[-1]] = 1
        self.eps_ap = self.cvec[:, 0:1]
        self.one_ap = self.cvec[:, 1:2]
        self.nln16_ap = self.cvec[:, 2:3]

    def scope(self):
        return Scope(self)

    def _deps(self, reads, writes, pwrites):
        deps = {}

        def addall(d):
            for k, v in d.items():
                if deps.get(k, 0) < v:
                    deps[k] = v
        for b in reads:
            addall(b.w)
            if b.excl:
                addall(b.r)
        for b in writes:
            addall(b.w)
            addall(b.r)
            addall(b.p)
        for b in pwrites:
            if b.r:
                addall(b.w)
                addall(b.r)
            addall(b.p)
            addall(b.wf)
        return deps

    def _waits(self, eng, deps):
        cur = eng.sems[-1]
        for k, v in deps.items():
            if k is cur and eng is self.pe:
                continue
            if eng.known.get(k, 0) >= v:
                continue
            eng.obj.wait_ge(k, v)
            eng.known[k] = v
            self.n_wait += 1

    def _record(self, ev, reads, writes, pwrites):
        k, v = ev
        for b in writes:
            p = dict(b.w)
            for kk, vv in b.r.items():
                if p.get(kk, 0) < vv:
                    p[kk] = vv
            b.p = p
            b.w = {k: v}
            b.wf = {k: v}
            b.r = {}
        for b in pwrites:
            if b.r:
                p = dict(b.w)
                for kk, vv in b.r.items():
                    if p.get(kk, 0) < vv:
                        p[kk] = vv
                b.p = p
                b.w = {k: v}
                b.wf = {}
                b.r = {}
            elif b.w.get(k, 0) < v:
                b.w[k] = v
        for b in reads:
            if b.r.get(k, 0) < v:
                b.r[k] = v

    def op(self, eng, fn, reads=(), writes=(), pwrites=(), inc=True):
        if eng.cnt >= self.SEM_EPOCH and not eng.pending:
            eng.new_sem()
        self._waits(eng, self._deps(reads, writes, pwrites))
        inst = fn(eng.obj)
        self.n_instr += 1
        if inc:
            eng.cnt += 1
            inst.then_inc(eng.sems[-1], 1)
            ev = (eng.sems[-1], eng.cnt)
            self._record(ev, reads, writes, pwrites)
            for (r, w, pw) in eng.pending:
                self._record(ev, r, w, pw)
            eng.pending = []
        else:
            ev = (eng.sems[-1], eng.cnt + 1)
            self._record(ev, reads, writes, pwrites)
            eng.pending.append((tuple(reads), tuple(writes), tuple(pwrites)))

    def dma(self, out, in_, reads=(), writes=(), pwrites=(), eng=None, **kw):
        eng = eng or self.sp
        if eng is self.pool:
            i = self.n_hw + self.dma_j % (len(self.dma_sems) - self.n_hw)
            self.dma_j += 1
        else:
            i = self.dma_i % self.n_hw
            self.dma_i += 1
        sem = self.dma_sems[i]
        deps = self._deps(reads, writes, pwrites)
        if self.dma_cnt[i] > 0 and deps.get(sem, 0) < self.dma_cnt[i]:
            deps[sem] = self.dma_cnt[i]
        self._waits(eng, deps)
        inst = eng.obj.dma_start(out=out, in_=in_, **kw)
        self.n_instr += 1
        self.dma_cnt[i] += 16
        inst.then_inc(sem, 16)
        self._record((sem, self.dma_cnt[i]), reads, writes, pwrites)

    def barrier(self):
        for e in self.engs:
            assert not e.pending
        allev = {}
        for e in self.engs:
            if e.cnt > 0:
                allev[e.sems[-1]] = e.cnt
        for i, s in enumerate(self.dma_sems):
            if self.dma_cnt[i] > 0:
                allev[s] = self.dma_cnt[i]
        for e in self.engs:
            self._waits(e, allev)
        for b in self.bufs:
            b.w = {}
            b.r = {}
            b.p = {}
            b.wf = {}

    def finish(self):
        self.barrier()
        self.stack.close()

    def mm(self, out, pairs, reads, wbuf, pw=False):
        n = len(pairs)
        for i, (l, r) in enumerate(pairs):
            self.op(self.pe, lambda e, l=l, r=r, i=i: e.matmul(out, lhsT=l, rhs=r, start=(i == 0), stop=(i == n - 1)),
                    reads=reads, writes=() if pw else (wbuf,), pwrites=(wbuf,) if pw else (), inc=(i == n - 1))

    def tr(self, out, in_, ident, reads, wbuf, last=True, pw=True):
        self.op(self.pe, lambda e: e.transpose(out=out, in_=in_, identity=ident), reads=reads,
                writes=() if pw else (wbuf,), pwrites=(wbuf,) if pw else (), inc=last)

    def acti(self, out, in_, func, reads, writes=(), pwrites=(), **kw):
        self.op(self.act, lambda e: e.activation(out=out, in_=in_, func=func, **kw), reads=reads, writes=writes, pwrites=pwrites)

    def copy_any(self, out, in_, reads, writes=(), pwrites=()):
        self.rr += 1
        if self.rr % 2:
            self.op(self.act, lambda e: e.copy(out=out, in_=in_), reads=reads, writes=writes, pwrites=pwrites)
        else:
            self.op(self.dve, lambda e: e.tensor_copy(out=out, in_=in_), reads=reads, writes=writes, pwrites=pwrites)

    def tt(self, eng, out, in0, in1, op, reads, writes=(), pwrites=()):
        self.op(eng, lambda e: e.tensor_tensor(out=out, in0=in0, in1=in1, op=op), reads=reads, writes=writes, pwrites=pwrites)

    def ts(self, eng, out, in0, s1, s2, op0, op1=None, reads=(), writes=(), pwrites=()):
        if op1 is None:
            self.op(eng, lambda e: e.tensor_scalar(out=out, in0=in0, scalar1=s1, scalar2=None, op0=op0), reads=reads, writes=writes, pwrites=pwrites)
        else:
            self.op(eng, lambda e: e.tensor_scalar(out=out, in0=in0, scalar1=s1, scalar2=s2, op0=op0, op1=op1), reads=reads, writes=writes, pwrites=pwrites)

    def rstd(self, out, in_, scale, buf):
        self.op(self.act, lambda e: e.activation(out=out, in_=in_, func=AF.Sqrt, scale=scale, bias=self.eps_ap), reads=[buf], pwrites=[buf])
        self.op(self.dve, lambda e: e.reciprocal(out=out, in_=out), reads=[buf], pwrites=[buf])

    def stt(self, eng, out, in0, scalar, in1, op0, op1, reads, writes=(), pwrites=()):
        self.op(eng, lambda e: e.scalar_tensor_tensor(out=out, in0=in0, scalar=scalar, in1=in1, op0=op0, op1=op1),
                reads=reads, writes=writes, pwrites=pwrites)


def bcast_rows(ap_row, n=128):
    a = ap_row.partition_broadcast(n)
    if len(a.shape) == 3:
        a = a.rearrange("p o f -> p (o f)")
    return a


CO_ID, CO_ONES, CO_TRIF, CO_TRIB, CO_RM, CO_NEGF, CO_NEGB, CO_NSEL = 0, 128, 256, 384, 512, 640, 768, 896
NCONST = 896 + 2048


def make_consts():
    c = np.zeros((128, NCONST), np.float32)
    i = np.arange(128)
    c[:, CO_ID:CO_ID + 128] = np.eye(128)
    c[:, CO_ONES:CO_ONES + 128] = 1.0
    c[:, CO_TRIF:CO_TRIF + 128] = (i[:, None] <= i[None, :])
    c[:, CO_TRIB:CO_TRIB + 128] = (i[:, None] >= i[None, :])
    r = np.zeros((128, 128), np.float32)
    r[2 * np.arange(64), 2 * np.arange(64) + 1] = 1.0
    r[2 * np.arange(64) + 1, 2 * np.arange(64)] = -1.0
    c[:, CO_RM:CO_RM + 128] = r
    c[:, CO_NEGF:CO_NEGF + 128] = np.where(i[:, None] <= i[None, :], 0.0, -30000.0)
    c[:, CO_NEGB:CO_NEGB + 128] = np.where(i[:, None] >= i[None, :], 0.0, -30000.0)
    ns = np.zeros((16, 16, 128), np.float32)
    ns[np.arange(16), np.arange(16), :] = 1.0
    c[0:16, CO_NSEL:] = ns.reshape(16, 2048)
    c[32:48, CO_NSEL:] = ns.reshape(16, 2048)
    return c


def make_rope():
    n_freq = 32
    inv = (10000.0 ** (-np.arange(n_freq, dtype=np.float32) / n_freq)).astype(np.float32)
    t = np.arange(SEQ)
    row = (t // 64).astype(np.float32)
    col = (t % 64).astype(np.float32)
    ang = np.concatenate([row[:, None] * inv, col[:, None] * inv], -1).astype(np.float32)
    cos = np.cos(ang).astype(np.float32)
    sin = np.sin(ang).astype(np.float32)
    cosT = np.repeat(cos.T, 2, axis=0)
    sinT = np.repeat(sin.T, 2, axis=0)
    return np.ascontiguousarray(cosT), np.ascontiguousarray(sinT)


class Prog:
    def __init__(self, debug=()):
        self.debug = set(debug)
        nc = bass.Bass("TRN2", target_bir_lowering=False)
        self.nc = nc
        self.fw = FW(nc)

        def din(name, shape, dt=F32):
            return nc.dram_tensor(name, list(shape), dt, kind="ExternalInput").ap()

        def scr(name, shape, dt):
            if name in self.debug:
                t = nc.dram_tensor(name, list(shape), dt, kind="ExternalOutput").ap()
            else:
                t = nc.dram_tensor(name, list(shape), dt).ap()
            return t, Buf(name, self.fw)
        self.xin = din("xin", [2, SEQ, D])
        self.ctxin = din("ctxin", [2, CTX, D])
        self.cst = din("cst", [128, 16, 3])
        self.w_mod = din("w_mod", [2, D, 3 * D])
        self.w_in = din("w_in", [2, D, NIN])
        self.w_gate = din("w_gate", [2, D, 3 * D])
        self.w_branch = din("w_branch", [2, 3, 1024, D])
        self.w_out = din("w_out", [2, D, D])
        self.b_mod = din("b_mod", [2, 3 * D])
        self.g_pre = din("g_pre", [2, D])
        self.g_post = din("g_post", [2, D])
        self.b_gateT = din("b_gateT", [2, 128, 48])
        self.att_sink = din("att_sink", [2, 8])
        self.ml_gb = din("ml_gb", [2, 16])
        self.ml_norm = din("ml_norm", [2, 1024])
        self.conv_wT = din("conv_wT", [2, 128, 12, 5])
        self.conv_bT = din("conv_bT", [2, 128, 12])
        self.dt_bias = din("dt_bias", [2, 32])
        self.a_log = din("a_log", [2, 32])
        self.ssm_d = din("ssm_d", [2, 16])
        self.ssm_norm = din("ssm_norm", [2, 1024])
        self.consts = din("consts", [128, NCONST])
        self.cosT = din("cosT", [128, SEQ])
        self.sinT = din("sinT", [128, SEQ])
        self.out = nc.dram_tensor("out", [2, SEQ, D], F32, kind="ExternalOutput").ap()
        self.b_out = Buf("out", self.fw)
        self.mods, self.b_mods = scr("mods", [2, 3, 3 * D], F32)
        self.xs1, self.b_xs1 = scr("xs1", [2, T, D], F32)
        self.hTs, self.b_hTs = scr("hTs", [16, 128, T], BF16)
        self.QTs, self.b_QTs = scr("QTs", [8, 128, T], BF16)
        self.KTs, self.b_KTs = scr("KTs", [2, 128, T], BF16)
        self.Vs, self.b_Vs = scr("Vs", [T, 256], BF16)
        self.ZTs, self.b_ZTs = scr("ZTs", [8, 128, T], BF16)
        self.MQTs, self.b_MQTs = scr("MQTs", [8, 128, T], BF16)
        self.MKTs, self.b_MKTs = scr("MKTs", [8, 128, T], BF16)
        self.MKs, self.b_MKs = scr("MKs", [T, 1024], BF16)
        self.MVs, self.b_MVs = scr("MVs", [T, 1024], BF16)
        self.MOs, self.b_MOs = scr("MOs", [T, 1024], BF16)
        self.MZs, self.b_MZs = scr("MZs", [T, 1024], BF16)
        self.Gs, self.b_Gs = scr("Gs", [T, 16], F32)
        self.DTs, self.b_DTs = scr("DTs", [T, 32], F32)
        self.SXs, self.b_SXs = scr("SXs", [T, 1024], BF16)
        self.SBs, self.b_SBs = scr("SBs", [T, 256], BF16)
        self.SBTs, self.b_SBTs = scr("SBTs", [2, 128, T], BF16)
        self.SCTs, self.b_SCTs = scr("SCTs", [2, 128, T], BF16)
        self.SZs, self.b_SZs = scr("SZs", [T, 1024], BF16)
        self.OTs, self.b_OTs = scr("OTs", [3, 8, 128, T], BF16)
        self.HFs, self.b_HFs = scr("HFs", [T, 1024], F32)
        self.YFs, self.b_YFs = scr("YFs", [T, 1024], F32)
        self.rows, self.b_rows = scr("rows", [NT, 2, 2, 2048], BF16)
        self.wg16, self.b_wg16 = scr("wg16", [12, 128, 16 * 512], BF16)
        self.wb16, self.b_wb16 = scr("wb16", [12, 128, 8 * 512], BF16)
        self.wo16, self.b_wo16 = scr("wo16", [4, 128, 16 * 512], BF16)

    def load_consts(self, sc, names):
        fw = self.fw
        out = {}
        for key, (off, width, dt, rows) in names.items():
            t, b = sc.tile("c_" + key, [rows, width], dt)
            if dt == BF16:
                fw.dma(t[:], self.consts[0:rows, off:off + width], writes=[b], eng=fw.pool)
            else:
                fw.dma(t[:], self.consts[0:rows, off:off + width], writes=[b])
            out[key] = (t, b)
        return out

    def phase_mod(self):
        fw = self.fw
        with fw.scope() as sc:
            cs, b_cs = sc.tile("cs", [128, 16, 3], F32)
            scT, b_scT = sc.tile("scT", [128, 16, 3], BF16)
            fw.dma(cs[:], self.cst, writes=[b_cs])
            fw.acti(scT[:], cs[:], AF.Silu, reads=[b_cs], writes=[b_scT])
            wr = sc.ring("wm", 2, [128, 16, 512], BF16)
            pr = sc.ring("pm", 2, [128, 512], F32, psum=True)
            br = sc.ring("bm", 2, [3, 512], F32)
            mr = sc.ring("mm", 2, [3, 512], F32)
            for l in range(2):
                for cb in range(12):
                    w, b_w = wr.next()
                    fw.dma(w[:], self.w_mod[l, :, cb * 512:(cb + 1) * 512].rearrange("(k p) c -> p k c", p=128),
                           writes=[b_w], eng=fw.pool)
                    bm, b_bm = br.next()
                    fw.dma(bm[:], bcast_rows(self.b_mod[l:l + 1, cb * 512:(cb + 1) * 512], 3), writes=[b_bm])
                    ps, b_ps = pr.next()
                    fw.mm(ps[0:3, :], [(scT[:, k, :], w[:, k, :]) for k in range(16)], [b_scT, b_w], b_ps)
                    m, b_m = mr.next()
                    fw.tt(fw.dve, m[:], ps[0:3, :], bm[:], ALU.add, reads=[b_ps, b_bm], writes=[b_m])
                    fw.dma(self.mods[l, :, cb * 512:(cb + 1) * 512], m[:], reads=[b_m], pwrites=[self.b_mods])

    def src_tile(self, l, s, i):
        if l == 0:
            if i < 2:
                return self.ctxin[s, i * 128:(i + 1) * 128, :]
            return self.xin[s, (i - 2) * 128:(i - 1) * 128, :]
        return self.xs1[s, i * 128:(i + 1) * 128, :]

    def phase_norm(self, sc, l, s, hT, b_hT, cn):
        fw = self.fw
        ident, b_id = cn["ident"]
        with fw.scope() as s2:
            rows = {}
            for j, nm in ((s, "x"), (2, "c")):
                s1, b_s1 = s2.tile("s1" + nm, [128, D], F32)
                sh, b_sh = s2.tile("sh" + nm, [128, D], F32)
                rows[nm] = (s1, b_s1, sh, b_sh)
            with fw.scope() as s3:
                gpre, b_gpre = s3.tile("gpre", [128, D], F32)
                fw.dma(gpre[:], bcast_rows(self.g_pre[l:l + 1, :]), writes=[b_gpre])
                tmp, b_tmp = s3.tile("sctmp", [128, D], F32)
                for j, nm in ((s, "x"), (2, "c")):
                    s1, b_s1, sh, b_sh = rows[nm]
                    fw.dma(tmp[:], bcast_rows(self.mods[l, j:j + 1, D:2 * D]), reads=[self.b_mods], writes=[b_tmp])
                    fw.dma(sh[:], bcast_rows(self.mods[l, j:j + 1, 0:D]), reads=[self.b_mods], writes=[b_sh])
                    fw.stt(fw.dve, s1[:], tmp[:], 1.0, gpre[:], ALU.add, ALU.mult, reads=[b_tmp, b_gpre], writes=[b_s1])
            xr = s2.ring("xt", 3, [128, D], F32)
            jr = s2.ring("junk", 1, [128, D], BF16)
            ssr = s2.ring("ss", 4, [128, 2], F32)
            hnr = s2.ring("hn", 3, [128, D], F32)
            hbr = s2.ring("hb", 3, [128, D], BF16)
            ptr = s2.ring("ptr", 2, [128, 16, 128], BF16, psum=True)
            for i in range(NT):
                s1, b_s1, sh, b_sh = rows["c" if i < 2 else "x"]
                xt, b_xt = xr.next()
                src_b = [self.b_xs1] if l > 0 else []
                fw.dma(xt[:], self.src_tile(l, s, i), reads=src_b, writes=[b_xt])
                jk, b_jk = jr.next()
                ss, b_ss = ssr.next()
                fw.acti(jk[:], xt[:], AF.Square, reads=[b_xt], writes=[b_jk, b_ss], accum_out=ss[:, 0:1])
                fw.rstd(ss[:, 1:2], ss[:, 0:1], 1.0 / D, b_ss)
                hn, b_hn = hnr.next()
                fw.stt(fw.dve, hn[:], xt[:], ss[:, 1:2], s1[:], ALU.mult, ALU.mult, reads=[b_xt, b_ss, b_s1], writes=[b_hn])
                hb, b_hb = hbr.next()
                fw.tt(fw.pool, hb[:], hn[:], sh[:], ALU.add, reads=[b_hn, b_sh], writes=[b_hb])
                pt, b_pt = ptr.next()
                for k in range(16):
                    fw.tr(pt[:, k, :], hb[:, k * 128:(k + 1) * 128], ident[:], [b_hb, b_id], b_pt, last=(k == 15), pw=(k > 0))
                fw.copy_any(hT[:, :, i * 128:(i + 1) * 128], pt[:], reads=[b_pt], pwrites=[b_hT])
        for k in range(16):
            fw.dma(self.hTs[k], hT[:, k, :], reads=[b_hT], pwrites=[self.b_hTs])

    def phase_proj(self, l, s):
        fw = self.fw
        with fw.scope() as sc:
            cn = self.load_consts(sc, {"ident": (CO_ID, 128, BF16, 128), "rmat": (CO_RM, 128, BF16, 128)})
            ident, b_id = cn["ident"]
            rmat, b_rm = cn["rmat"]
            hT, b_hT = sc.tile("hT", [128, 16, T], BF16)
            self.phase_norm(sc, l, s, hT, b_hT, cn)
            import os as _os
            CUT = _os.environ.get("CUT", "")
            if CUT == "norm":
                return
            groups = [(0, 256)] + [(256 + 512 * m, 512) for m in range(4)]
            wr = sc.ring("wblk", 3, [128, 16, 512], BF16)
            pr = sc.ring("pp", 3, [128, 512], F32, psum=True)

            def load_w(c0, ncols):
                w, b_w = wr.next()
                fw.dma(w[:, :, 0:ncols], self.w_in[l, :, c0:c0 + ncols].rearrange("(k p) c -> p k c", p=128),
                       writes=[b_w], eng=fw.pool)
                return w, b_w

            with fw.scope() as s2:
                cos, b_cos = s2.tile("cos", [128, SEQ], F32)
                sin, b_sin = s2.tile("sin", [128, SEQ], F32)
                fw.dma(cos[:], self.cosT, writes=[b_cos])
                fw.dma(sin[:], self.sinT, writes=[b_sin])
                sgr = s2.ring("stg", 3, [128, 512], BF16)
                qsr = s2.ring("qs", 2, [128, 512], BF16)
                t1r = s2.ring("t1", 2, [128, 512], F32)
                t2r = s2.ring("t2", 2, [128, 512], F32)
                p2r = s2.ring("pr", 2, [128, 512], F32, psum=True)
                fam = [("q", C_AQ, 8, self.QTs, self.b_QTs), ("q", C_AK, 2, self.KTs, self.b_KTs),
                       ("z", C_AZ, 8, self.ZTs, self.b_ZTs), ("c", C_MQ, 8, self.MQTs, self.b_MQTs),
                       ("c", C_MK, 8, self.MKTs, self.b_MKTs)]
                FAM = _os.environ.get("FAM", "")
                if FAM:
                    fam = [fam[int(c)] for c in FAM]
                for kind, c0, nch, dst, b_dst in fam:
                    for cb in range(0, nch, 4):
                        nb = min(4, nch - cb)
                        w, b_w = load_w(c0 + cb * 128, nb * 128)
                        for j in range(nb):
                            ch = cb + j
                            for (g0, gs) in groups:
                                ps, b_ps = pr.next()
                                fw.mm(ps[:, 0:gs], [(w[:, k, j * 128:(j + 1) * 128], hT[:, k, g0:g0 + gs]) for k in range(16)],
                                      [b_w, b_hT], b_ps)
                                st, b_st = sgr.next()
                                if kind == "z":
                                    fw.acti(st[:, 0:gs], ps[:, 0:gs], AF.Silu, reads=[b_ps], writes=[b_st])
                                elif kind == "c" or g0 < CTX:
                                    fw.copy_any(st[:, 0:gs], ps[:, 0:gs], reads=[b_ps], writes=[b_st])
                                else:
                                    x0 = g0 - CTX
                                    qs, b_qs = qsr.next()
                                    fw.op(fw.act, lambda e: e.copy(out=qs[:], in_=ps[:]), reads=[b_ps], writes=[b_qs])
                                    p2, b_p2 = p2r.next()
                                    fw.mm(p2[:], [(rmat[:], qs[:])], [b_rm, b_qs], b_p2)
                                    t1, b_t1 = t1r.next()
                                    t2, b_t2 = t2r.next()
                                    fw.tt(fw.dve, t1[:], ps[:], cos[:, x0:x0 + 512], ALU.mult, reads=[b_ps, b_cos], writes=[b_t1])
                                    fw.tt(fw.dve, t2[:], p2[:], sin[:, x0:x0 + 512], ALU.mult, reads=[b_p2, b_sin], writes=[b_t2])
                                    fw.tt(fw.pool, st[:], t1[:], t2[:], ALU.add, reads=[b_t1, b_t2], writes=[b_st])
                                fw.dma(dst[ch, :, g0:g0 + gs], st[:, 0:gs], reads=[b_st], pwrites=[b_dst])

            if CUT == "fm":
                return
            with fw.scope() as s2:
                cw, b_cw = s2.tile("cw", [128, 12, 5], F32)
                cb_, b_cb = s2.tile("cb", [128, 12], F32)
                fw.dma(cw[:], self.conv_wT[l], writes=[b_cw])
                fw.dma(cb_[:], self.conv_bT[l], writes=[b_cb])
                W = T + 8
                stripr = s2.ring("strip", 2, [128, W], F32)
                for (st_, b_s) in stripr.items:
                    fw.op(fw.pool, lambda e, st_=st_: e.memset(st_[:], 0.0), writes=[b_s])
                accr = s2.ring("acc", 2, [128, T + 4], F32)
                slr = s2.ring("sl", 3, [128, T + 4], BF16)
                ptr = s2.ring("ptx", 2, [128, 8, 128], BF16, psum=True)
                stgr = s2.ring("xstg", 2, [128, NT, 128], BF16)

                def tokcol(i):
                    return i * 128 if i < 2 else 260 + (i - 2) * 128
                deferred = []
                for cb in range(0, 12, 4):
                    w, b_w = load_w(C_SX + cb * 128, 512)
                    for j in range(4):
                        ch = cb + j
                        strip, b_strip = stripr.next()
                        first = True
                        for (g0, gs) in groups:
                            ps, b_ps = pr.next()
                            fw.mm(ps[:, 0:gs], [(w[:, k, j * 128:(j + 1) * 128], hT[:, k, g0:g0 + gs]) for k in range(16)],
                                  [b_w, b_hT], b_ps)
                            c0 = 2 + g0 if g0 < CTX else 262 + (g0 - CTX)
                            if first:
                                fw.copy_any(strip[:, c0:c0 + gs], ps[:, 0:gs], reads=[b_ps], writes=[b_strip])
                                first = False
                            else:
                                fw.copy_any(strip[:, c0:c0 + gs], ps[:, 0:gs], reads=[b_ps], pwrites=[b_strip])
                        while deferred:
                            deferred.pop(0)()
                        acc, b_acc = accr.next()
                        n = T + 4
                        fw.acti(acc[:], strip[:, 0:n], AF.Identity, reads=[b_strip, b_cw, b_cb], writes=[b_acc],
                                scale=cw[:, ch, 0:1], bias=cb_[:, ch:ch + 1])
                        for kk in range(1, 5):
                            fw.stt(fw.dve, acc[:], strip[:, kk:kk + n], cw[:, ch, kk:kk + 1], acc[:], ALU.mult, ALU.add,
                                   reads=[b_strip, b_cw, b_acc], pwrites=[b_acc])
                        sl, b_sl = slr.next()
                        fw.acti(sl[:], acc[:], AF.Silu, reads=[b_acc], writes=[b_sl])
                        if ch >= 8:
                            dst, b_dst = (self.SBTs, self.b_SBTs) if ch < 10 else (self.SCTs, self.b_SCTs)
                            g = (ch - 8) % 2
                            fw.dma(dst[g, :, 0:CTX], sl[:, 0:CTX], reads=[b_sl], pwrites=[b_dst])
                            fw.dma(dst[g, :, CTX:T], sl[:, 260:260 + SEQ], reads=[b_sl], pwrites=[b_dst])
                        if ch < 10:
                            def emit_tr(ch=ch, sl=sl, b_sl=b_sl):
                                stg, b_stg = stgr.next()
                                for i0 in range(0, NT, 8):
                                    nn = min(8, NT - i0)
                                    pt, b_pt = ptr.next()
                                    for ii in range(nn):
                                        c = tokcol(i0 + ii)
                                        fw.tr(pt[:, ii, :], sl[:, c:c + 128], ident[:], [b_sl, b_id], b_pt, last=(ii == nn - 1), pw=(ii > 0))
                                    if i0 == 0:
                                        fw.copy_any(stg[:, i0:i0 + nn, :], pt[:, 0:nn, :], reads=[b_pt], writes=[b_stg])
                                    else:
                                        fw.copy_any(stg[:, i0:i0 + nn, :], pt[:, 0:nn, :], reads=[b_pt], pwrites=[b_stg])
                                if ch < 8:
                                    fw.dma(self.SXs[:, ch * 128:(ch + 1) * 128].rearrange("(i p) c -> p i c", p=128), stg[:],
                                           reads=[b_stg], pwrites=[self.b_SXs])
                                else:
                                    fw.dma(self.SBs[:, (ch - 8) * 128:(ch - 7) * 128].rearrange("(i p) c -> p i c", p=128), stg[:],
                                           reads=[b_stg], pwrites=[self.b_SBs])
                            deferred.append(emit_tr)
                while deferred:
                    deferred.pop(0)()

            if CUT == "xbc":
                return
            with fw.scope() as s2:
                sgr = s2.ring("tstg", 3, [128, 512], BF16)
                sfr = s2.ring("fstg", 2, [128, 32], F32)
                fam = [("c", C_AV, 256, self.Vs, self.b_Vs), ("c", C_MK, 1024, self.MKs, self.b_MKs),
                       ("c", C_MV, 1024, self.MVs, self.b_MVs), ("sig", C_MO, 1024, self.MOs, self.b_MOs),
                       ("silu", C_MZ, 1024, self.MZs, self.b_MZs), ("f", C_MG, 16, self.Gs, self.b_Gs),
                       ("f", C_SDT, 32, self.DTs, self.b_DTs), ("silu", C_SZ, 1024, self.SZs, self.b_SZs)]
                for kind, c0, ncols, dst, b_dst in fam:
                    for cb in range(0, ncols, 512):
                        nb = min(512, ncols - cb)
                        w, b_w = load_w(c0 + cb, nb)
                        for i in range(NT):
                            ps, b_ps = pr.next()
                            fw.mm(ps[:, 0:nb], [(hT[:, k, i * 128:(i + 1) * 128], w[:, k, 0:nb]) for k in range(16)],
                                  [b_w, b_hT], b_ps)
                            if kind == "f":
                                st, b_st = sfr.next()
                                fw.copy_any(st[:, 0:nb], ps[:, 0:nb], reads=[b_ps], writes=[b_st])
                            else:
                                st, b_st = sgr.next()
                                if kind == "c":
                                    fw.copy_any(st[:, 0:nb], ps[:, 0:nb], reads=[b_ps], writes=[b_st])
                                else:
                                    fw.acti(st[:, 0:nb], ps[:, 0:nb], AF.Sigmoid if kind == "sig" else AF.Silu,
                                            reads=[b_ps], writes=[b_st])
                            fw.dma(dst[i * 128:(i + 1) * 128, cb:cb + nb], st[:, 0:nb], reads=[b_st], pwrites=[b_dst])

    def phase_att(self, l, s):
        fw = self.fw
        scale = 128.0 ** -0.5
        with fw.scope() as sc:
            cn = self.load_consts(sc, {"ones": (CO_ONES, 128, BF16, 128), "triF": (CO_TRIF, 128, BF16, 128),
                                       "triB": (CO_TRIB, 128, BF16, 128)})
            ones, b_ones = cn["ones"]
            KT, b_KT = sc.tile("KT", [128, 2, T], BF16)
            V, b_V = sc.tile("V", [128, NT, 256], BF16)
            fw.dma(KT[:], self.KTs.rearrange("g p t -> p g t"), reads=[self.b_KTs], writes=[b_KT])
            fw.dma(V[:], self.Vs.rearrange("(i p) c -> p i c", p=128), reads=[self.b_Vs], writes=[b_V])
            snk, b_snk = sc.tile("snk", [128, 8], F32)
            esk, b_esk = sc.tile("esk", [128, 8, 128], F32)
            fw.dma(snk[:], bcast_rows(self.att_sink[l:l + 1, :]), writes=[b_snk])
            fw.acti(snk[:], snk[:], AF.Exp, reads=[b_snk], writes=[b_snk])
            fw.op(fw.dve, lambda e: e.tensor_copy(out=esk[:], in_=snk[:].unsqueeze(2).to_broadcast([128, 8, 128])),
                  reads=[b_snk], writes=[b_esk])
            qr = sc.ring("qT", 2, [128, 8, 128], BF16)
            zr = sc.ring("zT", 2, [128, 8, 128], BF16)
            osr = sc.ring("ost", 2, [128, 8, 128], BF16)
            psr = sc.ring("pss", 3, [128, 512], F32, psum=True)
            por = sc.ring("pso", 2, [128, 512], F32, psum=True)
            pdr = sc.ring("psd", 2, [128, 512], F32, psum=True)
            ptr_ = sc.ring("pT", 6, [128, 512], BF16)
            ddr = sc.ring("dd", 2, [128, 512], F32)
            tor = sc.ring("to", 2, [128, 512], F32)
            tiles = list(range(2, NT)) + ([0, 1] if l == 0 else [])
            for ti in tiles:
                if ti >= 2:
                    keys = []
                    if ti > 2:
                        keys.append((ti - 1, "triB"))
                    keys.append((ti, None))
                    if ti < NT - 1:
                        keys.append((ti + 1, "triF"))
                    keys += [(0, None), (1, None)]
                else:
                    keys = [(0, None), (1, None)]
                qT, b_qT = qr.next()
                zT, b_zT = zr.next()
                fw.dma(qT[:], self.QTs[:, :, ti * 128:(ti + 1) * 128].rearrange("h p t -> p h t"), reads=[self.b_QTs], writes=[b_qT])
                fw.dma(zT[:], self.ZTs[:, :, ti * 128:(ti + 1) * 128].rearrange("h p t -> p h t"), reads=[self.b_ZTs], writes=[b_zT])
                ost, b_ost = osr.next()
                for g in range(2):
                    q2 = qT[:, 4 * g:4 * g + 4, :].rearrange("p a b -> p (a b)")
                    pts = []
                    for (kt, msk) in keys:
                        ps, b_ps = psr.next()
                        fw.mm(ps[:], [(KT[:, g, kt * 128:(kt + 1) * 128], q2)], [b_KT, b_qT], b_ps)
                        pT, b_pT = ptr_.next()
                        fw.acti(pT[:], ps[:], AF.Exp, reads=[b_ps], writes=[b_pT], scale=scale)
                        if msk is not None:
                            m, b_m = cn[msk]
                            p3 = pT[:].rearrange("p (a b) -> p a b", a=4)
                            fw.tt(fw.dve, p3, p3, m[:].unsqueeze(1).to_broadcast([128, 4, 128]), ALU.mult,
                                  reads=[b_pT, b_m], writes=[b_pT])
                        pts.append((kt, pT, b_pT))
                    po, b_po = por.next()
                    pd, b_pd = pdr.next()
                    fw.mm(po[:], [(V[:, kt, g * 128:(g + 1) * 128], pT[:]) for (kt, pT, _) in pts], [b_V] + [b for (_, _, b) in pts], b_po)
                    fw.mm(pd[:], [(ones[:], pT[:]) for (kt, pT, _) in pts], [b_ones] + [b for (_, _, b) in pts], b_pd)
                    dd, b_dd = ddr.next()
                    fw.tt(fw.dve, dd[:], pd[:], esk[:, 4 * g:4 * g + 4, :].rearrange("p a b -> p (a b)"), ALU.add,
                          reads=[b_pd, b_esk], writes=[b_dd])
                    fw.op(fw.dve, lambda e: e.reciprocal(out=dd[:], in_=dd[:]), reads=[b_dd], writes=[b_dd])
                    to, b_to = tor.next()
                    fw.tt(fw.dve, to[:], po[:], dd[:], ALU.mult, reads=[b_po, b_dd], writes=[b_to])
                    fw.tt(fw.pool, ost[:, 4 * g:4 * g + 4, :].rearrange("p a b -> p (a b)"), to[:],
                          zT[:, 4 * g:4 * g + 4, :].rearrange("p a b -> p (a b)"), ALU.mult,
                          reads=[b_to, b_zT], writes=[b_ost] if g == 0 else (), pwrites=() if g == 0 else [b_ost])
                fw.dma(self.OTs[0, :, :, ti * 128:(ti + 1) * 128].rearrange("h p t -> p h t"), ost[:], reads=[b_ost], pwrites=[self.b_OTs])

    def phase_mlstm(self, l, s):
        fw = self.fw
        with fw.scope() as sc:
            cn = self.load_consts(sc, {"ident": (CO_ID, 128, BF16, 128), "triFb": (CO_TRIF, 128, BF16, 128),
                                       "triBb": (CO_TRIB, 128, BF16, 128)})
            ident, b_id = cn["ident"]
            r_ = {}; c_ = {}; wd_ = {}; gd_ = {}
            bufs_g = []
            for d in range(2):
                for nm, dct in (("r", r_), ("c", c_), ("wd", wd_), ("gd", gd_)):
                    t, b = sc.tile(f"g{nm}{d}", [128, NT, 4], F32)
                    dct[d] = (t, b)
            with fw.scope() as s2:
                c2 = self.load_consts(s2, {"triF": (CO_TRIF, 128, F32, 128), "triB": (CO_TRIB, 128, F32, 128),
                                           "ones": (CO_ONES, 128, F32, 128)})
                Gt, b_Gt = s2.tile("Gt", [128, NT, 16], F32)
                gbias, b_gbias = s2.tile("gbias", [128, 16], F32)
                fw.dma(Gt[:], self.Gs.rearrange("(i p) c -> p i c", p=128), reads=[self.b_Gs], writes=[b_Gt])
                fw.dma(gbias[:], bcast_rows(self.ml_gb[l:l + 1, :]), writes=[b_gbias])
                fw.tt(fw.dve, Gt[:], Gt[:], gbias[:].unsqueeze(1).to_broadcast([128, NT, 16]), ALU.add,
                      reads=[b_Gt, b_gbias], writes=[b_Gt])
                for d in range(2):
                    ig = Gt[:, :, 8 * d:8 * d + 4]
                    fg = Gt[:, :, 8 * d + 4:8 * d + 8]
                    sp, b_sp = s2.tile(f"sp{d}", [128, NT, 4], F32)
                    a, b_a = s2.tile(f"a{d}", [128, NT, 4], F32)
                    fw.acti(sp[:], fg, AF.Exp, reads=[b_Gt], writes=[b_sp], scale=-1.0)
                    fw.acti(sp[:], sp[:], AF.Ln, reads=[b_sp], writes=[b_sp], bias=fw.one_ap)
                    pb, b_pb = s2.ptile(f"pb{d}", [128, 512], F32)
                    pt, b_pt = s2.ptile(f"ptot{d}", [128, 512], F32)
                    tri, b_tri = c2["triF" if d == 0 else "triB"]
                    on, b_on = c2["ones"]
                    sp2 = sp[:].rearrange("p a b -> p (a b)")
                    fw.mm(pb[:, 0:NT * 4], [(tri[:], sp2)], [b_tri, b_sp], b_pb)
                    fw.mm(pt[:, 0:NT * 4], [(on[:], sp2)], [b_on, b_sp], b_pt)
                    pb3 = pb[:, 0:NT * 4].rearrange("p (a b) -> p a b", b=4)
                    pt3 = pt[:, 0:NT * 4].rearrange("p (a b) -> p a b", b=4)
                    fw.acti(r_[d][0][:], pb3, AF.Exp, reads=[b_pb], writes=[r_[d][1]], scale=-1.0)
                    fw.acti(gd_[d][0][:], pt3, AF.Exp, reads=[b_pt], writes=[gd_[d][1]], scale=-1.0)
                    fw.tt(fw.dve, a[:], ig, pb3, ALU.add, reads=[b_Gt, b_pb], writes=[b_a])
                    fw.acti(c_[d][0][:], a[:], AF.Exp, reads=[b_a], writes=[c_[d][1]], bias=fw.nln16_ap)
                    fw.tt(fw.dve, a[:], a[:], pt3, ALU.subtract, reads=[b_a, b_pt], writes=[b_a])
                    fw.acti(wd_[d][0][:], a[:], AF.Exp, reads=[b_a], writes=[wd_[d][1]], bias=fw.nln16_ap)
            CstT = {}; CbfT = {}
            for d in range(2):
                for h in range(4):
                    CstT[d, h] = sc.tile(f"Cst{d}{h}", [128, 2, 257], F32)
                    CbfT[d, h] = sc.tile(f"Cbf{d}{h}", [128, 2, 257], BF16)
                    fw.op(fw.pool, lambda e, t=CstT[d, h][0]: e.memset(t[:], 0.0), writes=[CstT[d, h][1]])
                    fw.op(fw.pool, lambda e, t=CbfT[d, h][0]: e.memset(t[:], 0.0), writes=[CbfT[d, h][1]])
            nrow, b_nrow = sc.tile("nrow", [128, 1024], F32)
            fw.dma(nrow[:], bcast_rows(self.ml_norm[l:l + 1, :]), writes=[b_nrow])
            qr = sc.ring("mq", 4, [128, 8, 128], BF16)
            kr = sc.ring("mkT", 4, [128, 8, 128], BF16)
            k2r = sc.ring("mk", 4, [128, 1024], BF16)
            var = sc.ring("vaug", 4, [128, 4, 257], BF16)
            for (va, b_va) in var.items:
                fw.op(fw.pool, lambda e, va=va: e.memset(va[:], 1.0), writes=[b_va])
            hdr = sc.ring("hdir", 4, [128, 1024], F32)
            PTr = sc.ring("PT", 4, [128, 128], BF16)
            kwr = sc.ring("kw", 4, [128, 256], BF16)
            fr = sc.ring("fac", 4, [128, 2], F32)
            pss = sc.ring("ms", 2, [128, 512], F32, psum=True)
            psn = sc.ring("mn", 2, [128, 512], F32, psum=True)
            psc = sc.ring("mc", 2, [128, 512], F32, psum=True)
            ptr = sc.ring("mtr", 2, [128, 8, 128], BF16, psum=True)
            hfr = sc.ring("hf", 3, [128, 1024], F32)
            mor = sc.ring("mo", 3, [128, 1024], BF16)
            mzr = sc.ring("mz", 3, [128, 1024], BF16)
            hsr = sc.ring("hs", 2, [128, 1024], F32)
            nzr = sc.ring("nz", 2, [128, 1024], F32)
            jr = sc.ring("mj", 2, [128, 256], BF16)
            ssr = sc.ring("mss", 4, [128, 8], F32)
            omr = sc.ring("om", 4, [128, 1024], BF16)
            stg = sc.ring("mstg", 4, [128, 8, 128], BF16)

            def head(d, ti, h, qT, b_qT, kT, b_kT, k, b_k, va, b_va, hd, b_hd, mask, b_mask):
                Cst, b_Cst = CstT[d, h]
                Cbf, b_Cbf = CbfT[d, h]
                ps, b_ps = pss.next()
                fw.mm(ps[:, 0:128], [(kT[:, 2 * h + j, :], qT[:, 2 * h + j, :]) for j in range(2)], [b_kT, b_qT], b_ps)
                kw, b_kw = kwr.next()
                fw.acti(kw[:], k[:, h * 256:(h + 1) * 256], AF.Copy, reads=[b_k, wd_[d][1]], writes=[b_kw],
                        scale=wd_[d][0][:, ti, h:h + 1])
                yield
                PT, b_PT = PTr.next()
                fw.stt(fw.dve, PT[:], ps[:, 0:128], c_[d][0][:, ti, h:h + 1], mask[:], ALU.mult, ALU.mult,
                       reads=[b_ps, c_[d][1], b_mask], writes=[b_PT])
                yield
                pn, b_pn = psn.next()
                fw.mm(pn[:, 0:257], [(qT[:, 2 * h, :], Cbf[:, 0, :]), (qT[:, 2 * h + 1, :], Cbf[:, 1, :]),
                                     (PT[:], va[:, h, :])], [b_qT, b_Cbf, b_PT, b_va], b_pn)
                for j in range(2):
                    pc, b_pc = psc.next()
                    fw.mm(pc[:, 0:257], [(kw[:, j * 128:(j + 1) * 128], va[:, h, :])], [b_kw, b_va], b_pc)
                    fw.stt(fw.dve, Cst[:, j, :], Cst[:, j, :], gd_[d][0][:, ti, h:h + 1], pc[:, 0:257],
                           ALU.mult, ALU.add, reads=[b_pc, gd_[d][1], b_Cst], pwrites=[b_Cst])
                yield
                f, b_f = fr.next()
                rr = r_[d][0][:, ti, h:h + 1]
                fw.acti(f[:, 0:1], pn[:, 256:257], AF.Abs, reads=[b_pn, r_[d][1]], writes=[b_f], scale=rr)
                fw.op(fw.act, lambda e: e.copy(out=Cbf[:], in_=Cst[:]), reads=[b_Cst], writes=[b_Cbf])
                yield
                fw.ts(fw.dve, f[:, 0:1], f[:, 0:1], 1.0, None, ALU.max, reads=[b_f], writes=[b_f])
                fw.op(fw.dve, lambda e: e.reciprocal(out=f[:, 0:1], in_=f[:, 0:1]), reads=[b_f], writes=[b_f])
                fw.ts(fw.dve, f[:, 1:2], f[:, 0:1], rr, None, ALU.mult, reads=[b_f, r_[d][1]], writes=[b_f])
                yield
                fw.acti(hd[:, h * 256:(h + 1) * 256], pn[:, 0:256], AF.Copy, reads=[b_pn, b_f],
                        writes=[b_hd] if h == 0 else (), pwrites=() if h == 0 else [b_hd], scale=f[:, 1:2])

            def lockstep(gens):
                gens = list(gens)
                while gens:
                    for g in list(gens):
                        try:
                            next(g)
                        except StopIteration:
                            gens.remove(g)
                    yield

            def step(d, ti, second):
                mask, b_mask = cn["triFb" if d == 0 else "triBb"]
                qT, b_qT = qr.next(); kT, b_kT = kr.next(); k, b_k = k2r.next(); va, b_va = var.next()
                tsl = slice(ti * 128, (ti + 1) * 128)
                fw.dma(qT[:], self.MQTs[:, :, tsl].rearrange("h p t -> p h t"), reads=[self.b_MQTs], writes=[b_qT])
                fw.dma(kT[:], self.MKTs[:, :, tsl].rearrange("h p t -> p h t"), reads=[self.b_MKTs], writes=[b_kT])
                fw.dma(k[:], self.MKs[tsl, :], reads=[self.b_MKs], writes=[b_k])
                fw.dma(va[:, :, 0:256], self.MVs[tsl, :].rearrange("p (h e) -> p h e", h=4), reads=[self.b_MVs], writes=[b_va])
                fin = second and not (l == 1 and ti < 2)
                if fin:
                    hf, b_hf = hfr.next(); mo, b_mo = mor.next(); mz, b_mz = mzr.next()
                    fw.dma(hf[:], self.HFs[tsl, :], reads=[self.b_HFs], writes=[b_hf])
                    fw.dma(mo[:], self.MOs[tsl, :], reads=[self.b_MOs], writes=[b_mo])
                    fw.dma(mz[:], self.MZs[tsl, :], reads=[self.b_MZs], writes=[b_mz])
                hd, b_hd = hdr.next()
                yield
                for h in range(4):
                    yield from head(d, ti, h, qT, b_qT, kT, b_kT, k, b_k, va, b_va, hd, b_hd, mask, b_mask)
                    yield
                if l == 1 and ti < 2:
                    return
                if not second:
                    fw.dma(self.HFs[tsl, :], hd[:], reads=[b_hd], pwrites=[self.b_HFs])
                    return
                hs, b_hs = hsr.next(); nz, b_nz = nzr.next()
                fw.tt(fw.dve, hs[:], hf[:], hd[:], ALU.add, reads=[b_hf, b_hd], writes=[b_hs])
                fw.tt(fw.pool, nz[:], mz[:], nrow[:], ALU.mult, reads=[b_mz, b_nrow], writes=[b_nz])
                yield
                fw.tt(fw.dve, hs[:], hs[:], mo[:], ALU.mult, reads=[b_hs, b_mo], writes=[b_hs])
                yield
                ss, b_ss = ssr.next()
                jk, b_jk = jr.next()
                for h in range(4):
                    fw.acti(jk[:], hs[:, h * 256:(h + 1) * 256], AF.Square, reads=[b_hs], writes=[b_jk],
                            pwrites=[b_ss], accum_out=ss[:, h:h + 1])
                yield
                fw.rstd(ss[:, 4:8], ss[:, 0:4], 1.0 / 256, b_ss)
                yield
                om, b_om = omr.next()
                for h in range(4):
                    hsl = slice(h * 256, (h + 1) * 256)
                    fw.stt(fw.dve, om[:, hsl], hs[:, hsl], ss[:, 4 + h:5 + h], nz[:, hsl], ALU.mult, ALU.mult,
                           reads=[b_hs, b_ss, b_nz], writes=[b_om] if h == 0 else (), pwrites=() if h == 0 else [b_om])
                yield
                pt, b_pt = ptr.next()
                for c in range(8):
                    fw.tr(pt[:, c, :], om[:, c * 128:(c + 1) * 128], ident[:], [b_om, b_id], b_pt, last=(c == 7), pw=(c > 0))
                yield
                st, b_st = stg.next()
                fw.copy_any(st[:], pt[:], reads=[b_pt], writes=[b_st])
                fw.dma(self.OTs[1, :, :, tsl].rearrange("h p t -> p h t"), st[:], reads=[b_st], pwrites=[self.b_OTs])

            order = (list(range(NT)), [1, 0] + list(range(NT - 1, 1, -1)))
            seen = set()
            for n in range(NT):
                gens = []
                for d in range(2):
                    ti = order[d][n]
                    gens.append(step(d, ti, ti in seen))
                    seen.add(ti)
                for _ in lockstep(gens):
                    pass

    def phase_ssd(self, l, s):
        fw = self.fw
        with fw.scope() as sc:
            cn = self.load_consts(sc, {"ident": (CO_ID, 128, BF16, 128), "psel": (CO_NSEL, 2048, BF16, 48),
                                       "negF": (CO_NEGF, 128, BF16, 128), "negB": (CO_NEGB, 128, BF16, 128),
                                       "triF": (CO_TRIF, 128, F32, 128), "triB": (CO_TRIB, 128, F32, 128)})
            ident, b_id = cn["ident"]
            psel, b_psel = cn["psel"]
            neg4 = {}
            for d, nm in ((0, "negF"), (1, "negB")):
                t, b = sc.tile("neg4" + nm, [128, 4, 128], BF16)
                fw.op(fw.dve, lambda e, t=t, nm=nm: e.tensor_copy(out=t[:], in_=cn[nm][0][:].unsqueeze(1).to_broadcast([128, 4, 128])),
                      reads=[cn[nm][1]], writes=[b])
                neg4[d] = (t, b)
            nones, b_nones = sc.tile("nones", [2, 128], BF16)
            fw.op(fw.pool, lambda e: e.memset(nones[:], -1.0), writes=[b_nones])
            dt_ = {}; rc_ = {}; gd_ = {}; wd_ = {}; nla48 = {}
            for d in range(2):
                for nm, dct in (("dt", dt_), ("rc", rc_), ("gd", gd_), ("wd", wd_)):
                    dct[d] = sc.tile(f"s{nm}{d}", [128, NT, 16], F32)
                nla48[d] = sc.tile(f"nla48{d}", [128, NT, 48], F32)
            with fw.scope() as s2:
                c2 = self.load_consts(s2, {"ones": (CO_ONES, 128, F32, 128)})
                DTt, b_DTt = s2.tile("DTt", [128, NT, 32], F32)
                dtb, b_dtb = s2.tile("dtb", [128, 32], F32)
                ea, b_ea = s2.tile("ea", [128, 32], F32)
                fw.dma(DTt[:], self.DTs.rearrange("(i p) c -> p i c", p=128), reads=[self.b_DTs], writes=[b_DTt])
                fw.dma(dtb[:], bcast_rows(self.dt_bias[l:l + 1, :]), writes=[b_dtb])
                fw.dma(ea[:], bcast_rows(self.a_log[l:l + 1, :]), writes=[b_ea])
                fw.acti(ea[:], ea[:], AF.Exp, reads=[b_ea], writes=[b_ea])
                fw.tt(fw.dve, DTt[:], DTt[:], dtb[:].unsqueeze(1).to_broadcast([128, NT, 32]), ALU.add, reads=[b_DTt, b_dtb], writes=[b_DTt])
                fw.acti(DTt[:], DTt[:], AF.Exp, reads=[b_DTt], writes=[b_DTt])
                fw.acti(DTt[:], DTt[:], AF.Ln, reads=[b_DTt], writes=[b_DTt], bias=fw.one_ap)
                for d in range(2):
                    dt, b_dt = dt_[d]
                    fw.op(fw.dve, lambda e: e.tensor_copy(out=dt[:], in_=DTt[:, :, 16 * d:16 * d + 16]), reads=[b_DTt], writes=[b_dt])
                    nla, b_nla = s2.tile(f"nla{d}", [128, NT, 16], F32)
                    fw.tt(fw.dve, nla[:], dt[:], ea[:, 16 * d:16 * d + 16].unsqueeze(1).to_broadcast([128, NT, 16]), ALU.mult,
                          reads=[b_dt, b_ea], writes=[b_nla])
                    n48, b_n48 = nla48[d]
                    fw.op(fw.pool, lambda e: e.memset(n48[:], 0.0), writes=[b_n48])
                    fw.op(fw.pool, lambda e: e.tensor_copy(out=n48[:, :, 0:16], in_=nla[:]), reads=[b_nla], pwrites=[b_n48])
                    fw.op(fw.pool, lambda e: e.tensor_copy(out=n48[:, :, 32:48], in_=nla[:]), reads=[b_nla], pwrites=[b_n48])
                    pcu, b_pcu = s2.ptile(f"pcu{d}", [128, 512], F32)
                    pto, b_pto = s2.ptile(f"pto{d}", [128, 512], F32)
                    tri, b_tri = cn["triF" if d == 0 else "triB"]
                    on, b_on = c2["ones"]
                    n2 = nla[:].rearrange("p a b -> p (a b)")
                    fw.mm(pcu[:, 0:NT * 16], [(tri[:], n2)], [b_tri, b_nla], b_pcu)
                    fw.mm(pto[:, 0:NT * 16], [(on[:], n2)], [b_on, b_nla], b_pto)
                    pc3 = pcu[:, 0:NT * 16].rearrange("p (a b) -> p a b", b=16)
                    pt3 = pto[:, 0:NT * 16].rearrange("p (a b) -> p a b", b=16)
                    fw.acti(rc_[d][0][:], pc3, AF.Exp, reads=[b_pcu], writes=[rc_[d][1]], scale=-1.0)
                    fw.acti(gd_[d][0][:], pt3, AF.Exp, reads=[b_pto], writes=[gd_[d][1]], scale=-1.0)
                    tmp, b_tmp = s2.tile(f"stmp{d}", [128, NT, 16], F32)
                    fw.op(fw.dve, lambda e: e.tensor_copy(out=tmp[:], in_=pc3), reads=[b_pcu], writes=[b_tmp])
                    fw.tt(fw.dve, tmp[:], tmp[:], pt3, ALU.subtract, reads=[b_tmp, b_pto], writes=[b_tmp])
                    fw.acti(tmp[:], tmp[:], AF.Exp, reads=[b_tmp], writes=[b_tmp])
                    fw.tt(fw.dve, wd_[d][0][:], tmp[:], dt[:], ALU.mult, reads=[b_tmp, b_dt], writes=[wd_[d][1]])
            HstT = {}; HbfT = {}
            for d in range(2):
                HstT[d] = sc.tile(f"Hst{d}", [128, 1024], F32)
                HbfT[d] = sc.tile(f"Hbf{d}", [128, 1024], BF16)
                fw.op(fw.pool, lambda e, t=HstT[d][0]: e.memset(t[:], 0.0), writes=[HstT[d][1]])
                fw.op(fw.pool, lambda e, t=HbfT[d][0]: e.memset(t[:], 0.0), writes=[HbfT[d][1]])
            nrow, b_nrow = sc.tile("snrow", [128, 1024], F32)
            dsk, b_dsk = sc.tile("dsk", [128, 16], F32)
            fw.dma(nrow[:], bcast_rows(self.ssm_norm[l:l + 1, :]), writes=[b_nrow])
            fw.dma(dsk[:], bcast_rows(self.ssm_d[l:l + 1, :]), writes=[b_dsk])
            cTr = sc.ring("cT", 4, [48, 128], BF16)
            for (t, b) in cTr.items:
                fw.op(fw.pool, lambda e, t=t: e.memset(t[:], 0.0), writes=[b])
            h2r = sc.ring("hi2", 4, [48, 128], BF16)
            rwr = sc.ring("rw", 4, [2, 2048], BF16)
            xr = sc.ring("sx", 4, [128, 1024], BF16)
            Btr = sc.ring("sB", 4, [128, 256], BF16)
            BTr = sc.ring("sBT", 4, [128, 2, 128], BF16)
            CTr = sc.ring("sCT", 4, [128, 2, 128], BF16)
            CBr = sc.ring("CBs", 4, [128, 2, 128], BF16)
            Lr = sc.ring("Lb", 4, [128, 16, 128], BF16)
            MTr = sc.ring("MT", 4, [128, 16, 128], BF16)
            xdr = sc.ring("xdt", 4, [128, 1024], BF16)
            xwr = sc.ring("xw", 4, [128, 1024], BF16)
            ydr = sc.ring("ydir", 4, [128, 1024], F32)
            t1r = sc.ring("st1", 2, [128, 512], F32)
            psm = sc.ring("psm", 1, [128, 512], F32, psum=True)
            pse = sc.ring("pse", 2, [128, 512], F32, psum=True)
            pyr = sc.ring("psy", 1, [128, 2, 512], F32, psum=True)
            pir = sc.ring("psi", 1, [128, 2, 512], F32, psum=True)
            ptr = sc.ring("str", 1, [128, 8, 128], BF16, psum=True)
            yfr = sc.ring("yf", 4, [128, 1024], F32)
            szr = sc.ring("sz", 4, [128, 1024], BF16)
            ytr = sc.ring("yt", 2, [128, 1024], F32)
            jr = sc.ring("sj", 2, [128, 1024], BF16)
            ssr = sc.ring("sss", 4, [128, 2], F32)
            osr = sc.ring("os", 4, [128, 1024], BF16)
            stg = sc.ring("sstg", 4, [128, 8, 128], BF16)

            def step(d, ti, second):
                tri, b_tri = cn["triF" if d == 0 else "triB"]
                n4, b_n4 = neg4[d]
                Hst, b_Hst = HstT[d]
                Hbf, b_Hbf = HbfT[d]
                tsl = slice(ti * 128, (ti + 1) * 128)
                x, b_x = xr.next(); Bt, b_Bt = Btr.next(); BT, b_BT = BTr.next(); CT, b_CT = CTr.next()
                fw.dma(x[:], self.SXs[tsl, :], reads=[self.b_SXs], writes=[b_x])
                fw.dma(Bt[:], self.SBs[tsl, :], reads=[self.b_SBs], writes=[b_Bt])
                fw.dma(BT[:], self.SBTs[:, :, tsl].rearrange("g p t -> p g t"), reads=[self.b_SBTs], writes=[b_BT])
                fw.dma(CT[:], self.SCTs[:, :, tsl].rearrange("g p t -> p g t"), reads=[self.b_SCTs], writes=[b_CT])
                pc, b_pc = psm.next()
                n48, b_n48 = nla48[d]
                fw.mm(pc[0:48, 0:128], [(n48[:, ti, :], tri[:])], [b_n48, b_tri], b_pc)
                cT, b_cT = cTr.next(); h2, b_h2 = h2r.next()
                fw.op(fw.act, lambda e: e.copy(out=cT[0:16, :], in_=pc[0:16, 0:128]), reads=[b_pc], pwrites=[b_cT])
                fw.op(fw.act, lambda e: e.copy(out=h2[32:48, :], in_=pc[32:48, 0:128]), reads=[b_pc], writes=[b_h2])
                fw.tt(fw.dve, cT[32:48, :], pc[32:48, 0:128], h2[32:48, :], ALU.subtract, reads=[b_pc, b_h2], pwrites=[b_cT])
                fw.dma(self.rows[ti, d, 0, :].rearrange("(h t) -> h t", h=16), cT[0:16, :], reads=[b_cT], pwrites=[self.b_rows])
                fw.dma(self.rows[ti, d, 1, :].rearrange("(h t) -> h t", h=16), cT[32:48, :], reads=[b_cT], pwrites=[self.b_rows])
                rw, b_rw = rwr.next()
                fw.dma(rw[:], self.rows[ti, d], reads=[self.b_rows], writes=[b_rw])
                yield
                CB, b_CB = CBr.next()
                for g in range(2):
                    pcb, b_pcb = psm.next()
                    fw.mm(pcb[:, 0:128], [(BT[:, g, :], CT[:, g, :])], [b_BT, b_CT], b_pcb)
                    fw.op(fw.act, lambda e: e.copy(out=CB[:, g, :], in_=pcb[:, 0:128]), reads=[b_pcb],
                          writes=[b_CB] if g == 0 else (), pwrites=() if g == 0 else [b_CB])
                yield
                Lb, b_Lb = Lr.next()
                for bk in range(4):
                    pe_, b_pe = pse.next()
                    csl = slice(512 * bk, 512 * bk + 512)
                    fw.mm(pe_[:], [(nones[0:2, :], rw[0:2, csl]), (cT[0:48, :], psel[0:48, csl]),
                                   (ident[:], n4[:].rearrange("p a b -> p (a b)"))],
                          [b_nones, b_rw, b_cT, b_psel, b_id, b_n4], b_pe)
                    fw.acti(Lb[:, 4 * bk:4 * bk + 4, :].rearrange("p a b -> p (a b)"), pe_[:], AF.Exp, reads=[b_pe],
                            writes=[b_Lb] if bk == 0 else (), pwrites=() if bk == 0 else [b_Lb])
                yield
                MT, b_MT = MTr.next()
                for g in range(2):
                    fw.tt(fw.dve, MT[:, 8 * g:8 * g + 8, :], Lb[:, 8 * g:8 * g + 8, :],
                          CB[:, g, :].unsqueeze(1).to_broadcast([128, 8, 128]), ALU.mult,
                          reads=[b_Lb, b_CB], writes=[b_MT] if g == 0 else (), pwrites=() if g == 0 else [b_MT])
                xd, b_xd = xdr.next(); xw, b_xw = xwr.next()
                x3 = x[:].rearrange("p (h e) -> p h e", h=16)
                fw.tt(fw.pool, xd[:].rearrange("p (h e) -> p h e", h=16), x3,
                      dt_[d][0][:, ti, :].unsqueeze(2).to_broadcast([128, 16, 64]), ALU.mult, reads=[b_x, dt_[d][1]], writes=[b_xd])
                fw.tt(fw.pool, xw[:].rearrange("p (h e) -> p h e", h=16), x3,
                      wd_[d][0][:, ti, :].unsqueeze(2).to_broadcast([128, 16, 64]), ALU.mult, reads=[b_x, wd_[d][1]], writes=[b_xw])
                yield
                py, b_py = pyr.next()
                for h in range(16):
                    fw.op(fw.pe, lambda e, h=h: e.matmul(py[:, h // 8, (h % 8) * 64:(h % 8) * 64 + 64], lhsT=MT[:, h, :],
                                                          rhs=xd[:, h * 64:(h + 1) * 64], start=True, stop=True, skip_group_check=True),
                          reads=[b_MT, b_xd], writes=[b_py] if h == 0 else (), pwrites=() if h == 0 else [b_py], inc=(h == 15))
                pi, b_pi = pir.next()
                for g in range(2):
                    fw.mm(pi[:, g, :], [(CT[:, g, :], Hbf[:, g * 512:(g + 1) * 512])], [b_CT, b_Hbf], b_pi, pw=(g > 0))
                yd, b_yd = ydr.next()
                for g in range(2):
                    t1, b_t1 = t1r.next()
                    fw.tt(fw.dve, t1[:].rearrange("p (h e) -> p h e", h=8), pi[:, g, :].rearrange("p (h e) -> p h e", h=8),
                          rc_[d][0][:, ti, 8 * g:8 * g + 8].unsqueeze(2).to_broadcast([128, 8, 64]), ALU.mult,
                          reads=[b_pi, rc_[d][1]], writes=[b_t1])
                    fw.tt(fw.dve, yd[:, g * 512:(g + 1) * 512], py[:, g, :], t1[:], ALU.add, reads=[b_py, b_t1],
                          writes=[b_yd] if g == 0 else (), pwrites=() if g == 0 else [b_yd])
                yield
                ph, b_ph = pir.next()
                for g in range(2):
                    fw.mm(ph[:, g, :], [(Bt[:, g * 128:(g + 1) * 128], xw[:, g * 512:(g + 1) * 512])], [b_Bt, b_xw], b_ph, pw=(g > 0))
                for g in range(2):
                    hs3 = Hst[:, g * 512:(g + 1) * 512].rearrange("p (h e) -> p h e", h=8)
                    fw.tt(fw.dve, hs3, hs3, gd_[d][0][:, ti, 8 * g:8 * g + 8].unsqueeze(2).to_broadcast([128, 8, 64]), ALU.mult,
                          reads=[b_Hst, gd_[d][1]], pwrites=[b_Hst])
                    fw.tt(fw.dve, Hst[:, g * 512:(g + 1) * 512], ph[:, g, :], Hst[:, g * 512:(g + 1) * 512], ALU.add,
                          reads=[b_ph, b_Hst], pwrites=[b_Hst])
                fw.op(fw.act, lambda e: e.copy(out=Hbf[:], in_=Hst[:]), reads=[b_Hst], writes=[b_Hbf])
                yield
                if l == 1 and ti < 2:
                    return
                if not second:
                    fw.dma(self.YFs[tsl, :], yd[:], reads=[b_yd], pwrites=[self.b_YFs])
                    return
                yf, b_yf = yfr.next(); sz, b_sz = szr.next()
                fw.dma(yf[:], self.YFs[tsl, :], reads=[self.b_YFs], writes=[b_yf])
                fw.dma(sz[:], self.SZs[tsl, :], reads=[self.b_SZs], writes=[b_sz])
                yt, b_yt = ytr.next()
                fw.tt(fw.pool, yt[:].rearrange("p (h e) -> p h e", h=16), x3, dsk[:].unsqueeze(2).to_broadcast([128, 16, 64]), ALU.mult,
                      reads=[b_x, b_dsk], writes=[b_yt])
                fw.tt(fw.pool, yt[:], yt[:], yf[:], ALU.add, reads=[b_yt, b_yf], writes=[b_yt])
                fw.tt(fw.dve, yt[:], yt[:], yd[:], ALU.add, reads=[b_yt, b_yd], writes=[b_yt])
                fw.tt(fw.dve, yt[:], yt[:], sz[:], ALU.mult, reads=[b_yt, b_sz], writes=[b_yt])
                yield
                ss, b_ss = ssr.next(); jk, b_jk = jr.next()
                fw.acti(jk[:], yt[:], AF.Square, reads=[b_yt], writes=[b_jk, b_ss], accum_out=ss[:, 0:1])
                fw.rstd(ss[:, 1:2], ss[:, 0:1], 1.0 / 1024, b_ss)
                yield
                os_, b_os = osr.next()
                fw.stt(fw.dve, os_[:], yt[:], ss[:, 1:2], nrow[:], ALU.mult, ALU.mult, reads=[b_yt, b_ss, b_nrow], writes=[b_os])
                yield
                pt, b_pt = ptr.next()
                for c in range(8):
                    fw.tr(pt[:, c, :], os_[:, c * 128:(c + 1) * 128], ident[:], [b_os, b_id], b_pt, last=(c == 7), pw=(c > 0))
                st, b_st = stg.next()
                fw.copy_any(st[:], pt[:], reads=[b_pt], writes=[b_st])
                fw.dma(self.OTs[2, :, :, tsl].rearrange("h p t -> p h t"), st[:], reads=[b_st], pwrites=[self.b_OTs])

            def lockstep(gens):
                gens = list(gens)
                while gens:
                    for g in list(gens):
                        try:
                            next(g)
                        except StopIteration:
                            gens.remove(g)

            order = (list(range(NT)), [1, 0] + list(range(NT - 1, 1, -1)))
            seen = set()
            for n in range(NT):
                gens = []
                for d in range(2):
                    ti = order[d][n]
                    gens.append(step(d, ti, ti in seen))
                    seen.add(ti)
                lockstep(gens)

    def phase_merge(self, l, s):
        fw = self.fw
        with fw.scope() as sc:
            bgT, b_bgT = sc.tile("bgT", [128, 48], F32)
            fw.dma(bgT[:], self.b_gateT[l], writes=[b_bgT])
            g2 = {}
            for j, nm in ((s, "x"), (2, "c")):
                if nm == "c" and l == 1:
                    continue
                g2[nm] = sc.tile("g2" + nm, [128, D], F32)
            with fw.scope() as s3:
                gpost, b_gpost = s3.tile("gpost", [128, D], F32)
                fw.dma(gpost[:], bcast_rows(self.g_post[l:l + 1, :]), writes=[b_gpost])
                for j, nm in ((s, "x"), (2, "c")):
                    if nm == "c" and l == 1:
                        continue
                    t, b = g2[nm]
                    fw.dma(t[:], bcast_rows(self.mods[l, j:j + 1, 2 * D:3 * D]), reads=[self.b_mods], writes=[b])
                    fw.tt(fw.dve, t[:], t[:], gpost[:], ALU.mult, reads=[b, b_gpost], writes=[b])
            groups = ([(0, 256)] if l == 0 else []) + [(256 + 512 * m, 512) for m in range(4)]
            hg, b_hg = sc.tile("hg", [128, 16, 512], BF16)
            og, b_og = sc.tile("og", [128, 3, 8, 512], BF16)
            mT, b_mT = sc.tile("mT", [128, 16, 512], BF16)
            macc, b_macc = sc.tile("macc", [128, 4, 512], F32)
            wgr = sc.ring("wg", 3, [128, 16, 512], BF16)
            wbr = sc.ring("wb", 3, [128, 8, 512], BF16)
            wor = wgr
            sgr = sc.ring("sg", 2, [128, 512], F32)
            tmr = sc.ring("tm", 2, [128, 512], F32)
            pgr = sc.ring("pg", 3, [128, 512], F32, psum=True)
            ppr = sc.ring("ppj", 3, [128, 512], F32, psum=True)
            pyr = sc.ring("pyo", 2, [128, 512], F32, psum=True)
            ysb = [sc.tile(f"ysb{i}", [128, D], F32) for i in range(4)]
            xr = sc.ring("xres", 1, [128, D], F32)
            jr = sc.ring("ej", 1, [128, D], BF16)
            ssr = sc.ring("ess", 2, [128, 2], F32)
            for gi, (g0, gs) in enumerate(groups):
                G2, b_G2 = g2["c" if g0 < CTX else "x"]
                conv = (s == 0 and gi == 0)
                fw.dma(hg[:, :, 0:gs], self.hTs[:, :, g0:g0 + gs].rearrange("k p t -> p k t"), reads=[self.b_hTs], writes=[b_hg])
                for n in range(3):
                    fw.dma(og[:, n, :, 0:gs], self.OTs[n, :, :, g0:g0 + gs].rearrange("k p t -> p k t"), reads=[self.b_OTs],
                           writes=[b_og] if n == 0 else (), pwrites=() if n == 0 else [b_og])
                for cb in range(4):
                    for n in range(3):
                        wg, b_wg = wgr.next(); wb, b_wb = wbr.next()
                        c0 = n * D + cb * 512
                        if conv:
                            fw.dma(wg[:], self.w_gate[l, :, c0:c0 + 512].rearrange("(k p) c -> p k c", p=128), writes=[b_wg], eng=fw.pool)
                            fw.dma(wb[:], self.w_branch[l, n, :, cb * 512:(cb + 1) * 512].rearrange("(k p) c -> p k c", p=128),
                                   writes=[b_wb], eng=fw.pool)
                            fw.dma(self.wg16[n * 4 + cb], wg[:].rearrange("p k c -> p (k c)"), reads=[b_wg], pwrites=[self.b_wg16])
                            fw.dma(self.wb16[n * 4 + cb], wb[:].rearrange("p k c -> p (k c)"), reads=[b_wb], pwrites=[self.b_wb16])
                        else:
                            fw.dma(wg[:].rearrange("p k c -> p (k c)"), self.wg16[n * 4 + cb], reads=[self.b_wg16], writes=[b_wg])
                            fw.dma(wb[:].rearrange("p k c -> p (k c)"), self.wb16[n * 4 + cb], reads=[self.b_wb16], writes=[b_wb], eng=fw.pool)
                        for j in range(4):
                            ch = cb * 4 + j
                            pg, b_pg = pgr.next(); pp, b_pp = ppr.next()
                            fw.mm(pg[:, 0:gs], [(wg[:, k, j * 128:(j + 1) * 128], hg[:, k, 0:gs]) for k in range(16)], [b_wg, b_hg], b_pg)
                            fw.mm(pp[:, 0:gs], [(wb[:, k, j * 128:(j + 1) * 128], og[:, n, k, 0:gs]) for k in range(8)], [b_wb, b_og], b_pp)
                            sg, b_sg = sgr.next()
                            fw.acti(sg[:, 0:gs], pg[:, 0:gs], AF.Sigmoid, reads=[b_pg, b_bgT], writes=[b_sg],
                                    bias=bgT[:, n * 16 + ch:n * 16 + ch + 1])
                            if n == 0:
                                fw.tt(fw.dve, macc[:, j, 0:gs], sg[:, 0:gs], pp[:, 0:gs], ALU.mult, reads=[b_sg, b_pp],
                                      writes=[b_macc] if j == 0 else (), pwrites=() if j == 0 else [b_macc])
                            else:
                                tm, b_tm = tmr.next()
                                fw.tt(fw.dve, tm[:, 0:gs], sg[:, 0:gs], pp[:, 0:gs], ALU.mult, reads=[b_sg, b_pp], writes=[b_tm])
                                if n == 1:
                                    fw.tt(fw.pool, macc[:, j, 0:gs], macc[:, j, 0:gs], tm[:, 0:gs], ALU.add, reads=[b_tm, b_macc], pwrites=[b_macc])
                                else:
                                    first = (cb == 0 and j == 0)
                                    fw.tt(fw.pool, mT[:, ch, 0:gs], macc[:, j, 0:gs], tm[:, 0:gs], ALU.add, reads=[b_tm, b_macc],
                                          writes=[b_mT] if first else (), pwrites=() if first else [b_mT])
                nt = gs // 128
                for cb in range(4):
                    wo, b_wo = wor.next()
                    if conv:
                        fw.dma(wo[:], self.w_out[l, :, cb * 512:(cb + 1) * 512].rearrange("(k p) c -> p k c", p=128), writes=[b_wo], eng=fw.pool)
                        fw.dma(self.wo16[cb], wo[:].rearrange("p k c -> p (k c)"), reads=[b_wo], pwrites=[self.b_wo16])
                    else:
                        fw.dma(wo[:].rearrange("p k c -> p (k c)"), self.wo16[cb], reads=[self.b_wo16], writes=[b_wo])
                    for t in range(nt):
                        py, b_py = pyr.next()
                        fw.mm(py[:], [(mT[:, k, t * 128:(t + 1) * 128], wo[:, k, :]) for k in range(16)], [b_mT, b_wo], b_py)
                        yt, b_yt = ysb[t]
                        fw.copy_any(yt[:, cb * 512:(cb + 1) * 512], py[:], reads=[b_py],
                                    writes=[b_yt] if cb == 0 else (), pwrites=() if cb == 0 else [b_yt])
                for t in range(nt):
                    ti = (g0 + t * 128) // 128
                    yt, b_yt = ysb[t]
                    ss, b_ss = ssr.next(); jk, b_jk = jr.next()
                    fw.acti(jk[:], yt[:], AF.Square, reads=[b_yt], writes=[b_jk, b_ss], accum_out=ss[:, 0:1])
                    fw.rstd(ss[:, 1:2], ss[:, 0:1], 1.0 / D, b_ss)
                    xt, b_xt = xr.next()
                    fw.dma(xt[:], self.src_tile(l, s, ti), reads=[self.b_xs1] if l > 0 else [], writes=[b_xt])
                    fw.stt(fw.dve, yt[:], yt[:], ss[:, 1:2], G2[:], ALU.mult, ALU.mult, reads=[b_yt, b_ss, b_G2], writes=[b_yt])
                    fw.tt(fw.pool, xt[:], xt[:], yt[:], ALU.add, reads=[b_xt, b_yt], writes=[b_xt])
                    if l == 0:
                        fw.dma(self.xs1[s, ti * 128:(ti + 1) * 128, :], xt[:], reads=[b_xt], pwrites=[self.b_xs1])
                    else:
                        fw.dma(self.out[s, (ti - 2) * 128:(ti - 1) * 128, :], xt[:], reads=[b_xt], pwrites=[self.b_out])


def build(plan=None, debug=()):
    p = Prog(debug=debug)
    if plan is None:
        plan = [(l, s) for l in range(2) for s in range(2)]
    p.plan = plan
    p.phase_mod()
    stages = getattr(build, "stages", "PBCDE")
    for (l, s) in plan:
        if "P" in stages:
            p.phase_proj(l, s)
        if "B" in stages:
            p.phase_att(l, s)
        if "C" in stages:
            p.phase_mlstm(l, s)
        if "D" in stages:
            p.phase_ssd(l, s)
        if "E" in stages:
            p.phase_merge(l, s)
    p.fw.finish()
    return p


def fmaj(v, chunks):
    return np.ascontiguousarray(np.asarray(v, np.float32).reshape(chunks, 128).T)


def shared_inputs(inp):
    cosT, sinT = make_rope()
    sh = {
        "w_mod": inp["w_mod"], "w_in": inp["w_in"], "w_gate": inp["w_gate"], "w_branch": inp["w_branch"],
        "w_out": inp["w_out"], "b_mod": inp["b_mod"], "g_pre": inp["g_pre"], "g_post": inp["g_post"],
        "b_gateT": np.stack([fmaj(inp["b_gate"][l], 48) for l in range(2)]),
        "att_sink": inp["att_sink"], "ml_gb": np.asarray(inp["ml_gate_bias"], np.float32).reshape(2, 16),
        "ml_norm": inp["ml_norm"],
        "conv_wT": np.stack([np.ascontiguousarray(np.asarray(inp["ssm_conv_w"][l]).T.reshape(12, 128, 5).transpose(1, 0, 2))
                             for l in range(2)]),
        "conv_bT": np.stack([fmaj(inp["ssm_conv_b"][l], 12) for l in range(2)]),
        "dt_bias": np.asarray(inp["ssm_dt_bias"], np.float32).reshape(2, 32),
        "a_log": np.asarray(inp["ssm_a_log"], np.float32).reshape(2, 32),
        "ssm_d": inp["ssm_d"], "ssm_norm": inp["ssm_norm"],
        "consts": make_consts(), "cosT": cosT, "sinT": sinT,
    }
    return {k: np.ascontiguousarray(np.asarray(v, np.float32)) for k, v in sh.items()}


def core_inputs(inp, sh, core):
    b0 = 2 * core
    cst = np.stack([fmaj(inp["c"][b0], 16), fmaj(inp["c"][b0 + 1], 16), fmaj(inp["c_ctx"], 16)], axis=-1)
    m = dict(sh)
    m["xin"] = np.ascontiguousarray(np.asarray(inp["x"][b0:b0 + 2], np.float32))
    m["ctxin"] = np.ascontiguousarray(np.asarray(inp["ctx"][b0:b0 + 2], np.float32))
    m["cst"] = np.ascontiguousarray(cst.astype(np.float32))
    return m


def kernel(**inputs):
    p = build()
    sh = shared_inputs(inputs)
    in_maps = [core_inputs(inputs, sh, c) for c in range(NCORES)]
    res = run_bass_kernel_spmd(p.nc, in_maps, core_ids=list(range(NCORES)))
    return np.concatenate([np.asarray(r["out"], np.float32) for r in res.results], axis=0)
```

```python
import math
import numpy as np
import concourse.bass as bass
import concourse.mybir as mybir
from concourse.bass_utils import run_bass_kernel_spmd
from contextlib import ExitStack

F32 = mybir.dt.float32
BF16 = mybir.dt.bfloat16
AF = mybir.ActivationFunctionType
ALU = mybir.AluOpType
AX = mybir.AxisListType

D = 2048
SEQ = 2048
CTX = 256
T = SEQ + CTX
NT = T // 128
NIN = 10288
EPS = 1e-6
NCORES = 8
C_AQ, C_AK, C_AV, C_AZ = 0, 1024, 1280, 1536
C_MQ, C_MK, C_MV, C_MO, C_MZ, C_MG = 2560, 3584, 4608, 5632, 6656, 7680
C_SX, C_SDT, C_SZ = 7696, 9232, 9264


class Buf:
    __slots__ = ("name", "w", "r", "p", "wf", "excl")

    def __init__(self, name, fw=None, excl=False):
        self.name = name
        self.excl = excl
        self.w = {}
        self.r = {}
        self.p = {}
        self.wf = {}
        if fw is not None:
            fw.bufs.append(self)


class Eng:
    def __init__(self, fw, name, obj):
        self.fw = fw
        self.name = name
        self.obj = obj
        self.cnt = 0
        self.sems = []
        self.known = {}
        self.pending = []
        self.new_sem()

    def new_sem(self):
        s = self.fw.stack.enter_context(self.fw.nc.semaphore(f"s_{self.name}_{len(self.sems)}"))
        self.sems.append(s)
        self.cnt = 0


class Scope:
    def __init__(self, fw):
        self.fw = fw
        self.stack = ExitStack()

    def __enter__(self):
        return self

    def __exit__(self, *a):
        self.fw.barrier()
        self.stack.close()
        return False

    def sbuf(self, name, shape, dtype):
        self.fw.uid += 1
        return self.stack.enter_context(self.fw.nc.sbuf_tensor(f"{name}_{self.fw.uid}", list(shape), dtype))

    def psum(self, name, shape, dtype):
        self.fw.uid += 1
        return self.stack.enter_context(self.fw.nc.psum_tensor(f"{name}_{self.fw.uid}", list(shape), dtype))

    def tile(self, name, shape, dtype):
        return self.sbuf(name, shape, dtype), Buf(name, self.fw)

    def ptile(self, name, shape, dtype):
        nbytes = int(np.prod(shape[1:])) * (4 if dtype == F32 else 2)
        assert nbytes % 2048 == 0, (name, shape)
        return self.psum(name, shape, dtype), Buf(name, self.fw, excl=True)

    def ring(self, name, n, shape, dtype, psum=False):
        return Ring([(self.ptile if psum else self.tile)(f"{name}{i}", shape, dtype) for i in range(n)])


class Ring:
    def __init__(self, items):
        self.items = items
        self.i = 0

    def next(self):
        it = self.items[self.i % len(self.items)]
        self.i += 1
        return it


class FW:
    SEM_EPOCH = 30000

    def __init__(self, nc, n_dma_sems=32):
        self.nc = nc
        self.stack = ExitStack()
        self.bufs = []
        self.uid = 0
        self.pe = Eng(self, "pe", nc.tensor)
        self.act = Eng(self, "act", nc.scalar)
        self.dve = Eng(self, "dve", nc.vector)
        self.pool = Eng(self, "pool", nc.gpsimd)
        self.sp = Eng(self, "sp", nc.sync)
        self.engs = [self.pe, self.act, self.dve, self.pool, self.sp]
        self.dma_sems = [self.stack.enter_context(nc.semaphore(f"dq{i}")) for i in range(n_dma_sems)]
        self.dma_cnt = [0] * n_dma_sems
        self.n_hw = n_dma_sems - 12
        self.dma_i = 0
        self.dma_j = 0
        self.n_instr = 0
        self.n_wait = 0
        self.rr = 0
        self.cvec = self.stack.enter_context(nc.sbuf_tensor("cvec", [128, 4], F32))
        nc.gpsimd.memset(self.cvec[:, 0:1], EPS)
        nc.gpsimd.memset(self.cvec[:, 1:2], 1.0)
        nc.gpsimd.memset(self.cvec[:, 2:3], -math.log(16.0))
        nc.gpsimd.memset(self.cvec[:, 3:4], 0.0).then_inc(self.pool.sems[-1], 1)
        self.pool.cnt += 1
        for e in self.engs:
            if e is not self.pool:
                e.obj.wait_ge(self.pool.sems[-1], 1)
                e.known[self.pool.sems[-1]] = 1
        self.eps_ap = self.cvec[:, 0:1]
        self.one_ap = self.cvec[:, 1:2]
        self.nln16_ap = self.cvec[:, 2:3]

    def scope(self):
        return Scope(self)

    def _deps(self, reads, writes, pwrites):
        deps = {}

        def addall(d):
            for k, v in d.items():
                if deps.get(k, 0) < v:
                    deps[k] = v
        for b in reads:
            addall(b.w)
            if b.excl:
                addall(b.r)
        for b in writes:
            addall(b.w)
            addall(b.r)
            addall(b.p)
        for b in pwrites:
            if b.r:
                addall(b.w)
                addall(b.r)
            addall(b.p)
            addall(b.wf)
        return deps

    def _waits(self, eng, deps):
        cur = eng.sems[-1]
        for k, v in deps.items():
            if k is cur and eng is self.pe:
                continue
            if eng.known.get(k, 0) >= v:
                continue
            eng.obj.wait_ge(k, v)
            eng.known[k] = v
            self.n_wait += 1

    def _record(self, ev, reads, writes, pwrites):
        k, v = ev
        for b in writes:
            p = dict(b.w)
            for kk, vv in b.r.items():
                if p.get(kk, 0) < vv:
                    p[kk] = vv
            b.p = p
            b.w = {k: v}
            b.wf = {k: v}
            b.r = {}
        for b in pwrites:
            if b.r:
                p = dict(b.w)
                for kk, vv in b.r.items():
                    if p.get(kk, 0) < vv:
                        p[kk] = vv
                b.p = p
                b.w = {k: v}
                b.wf = {}
                b.r = {}
            elif b.w.get(k, 0) < v:
                b.w[k] = v
        for b in reads:
            if b.r.get(k, 0) < v:
                b.r[k] = v

    def op(self, eng, fn, reads=(), writes=(), pwrites=(), inc=True):
        if eng.cnt >= self.SEM_EPOCH and not eng.pending:
            eng.new_sem()
        self._waits(eng, self._deps(reads, writes, pwrites))
        inst = fn(eng.obj)
        self.n_instr += 1
        if inc:
            eng.cnt += 1
            inst.then_inc(eng.sems[-1], 1)
            ev = (eng.sems[-1], eng.cnt)
            self._record(ev, reads, writes, pwrites)
            for (r, w, pw) in eng.pending:
                self._record(ev, r, w, pw)
            eng.pending = []
        else:
            ev = (eng.sems[-1], eng.cnt + 1)
            self._record(ev, reads, writes, pwrites)
            eng.pending.append((tuple(reads), tuple(writes), tuple(pwrites)))

    def dma(self, out, in_, reads=(), writes=(), pwrites=(), eng=None, **kw):
        eng = eng or self.sp
        if eng is self.pool:
            i = self.n_hw + self.dma_j % (len(self.dma_sems) - self.n_hw)
            self.dma_j += 1
        else:
            i = self.dma_i % self.n_hw
            self.dma_i += 1
        sem = self.dma_sems[i]
        deps = self._deps(reads, writes, pwrites)
        if self.dma_cnt[i] > 0 and deps.get(sem, 0) < self.dma_cnt[i]:
            deps[sem] = self.dma_cnt[i]
        self._waits(eng, deps)
        inst = eng.obj.dma_start(out=out, in_=in_, **kw)
        self.n_instr += 1
        self.dma_cnt[i] += 16
        inst.then_inc(sem, 16)
        self._record((sem, self.dma_cnt[i]), reads, writes, pwrites)

    def barrier(self):
        for e in self.engs:
            assert not e.pending
        allev = {}
        for e in self.engs:
            if e.cnt > 0:
                allev[e.sems[-1]] = e.cnt
        for i, s in enumerate(self.dma_sems):
            if self.dma_cnt[i] > 0:
                allev[s] = self.dma_cnt[i]
        for e in self.engs:
            self._waits(e, allev)
        for b in self.bufs:
            b.w = {}
            b.r = {}
            b.p = {}
            b.wf = {}

    def finish(self):
        self.barrier()
        self.stack.close()

    def mm(self, out, pairs, reads, wbuf, pw=False):
        n = len(pairs)
        for i, (l, r) in enumerate(pairs):
            self.op(self.pe, lambda e, l=l, r=r, i=i: e.matmul(out, lhsT=l, rhs=r, start=(i == 0), stop=(i == n - 1)),
                    reads=reads, writes=() if pw else (wbuf,), pwrites=(wbuf,) if pw else (), inc=(i == n - 1))

    def tr(self, out, in_, ident, reads, wbuf, last=True, pw=True):
        self.op(self.pe, lambda e: e.transpose(out=out, in_=in_, identity=ident), reads=reads,
                writes=() if pw else (wbuf,), pwrites=(wbuf,) if pw else (), inc=last)

    def acti(self, out, in_, func, reads, writes=(), pwrites=(), **kw):
        self.op(self.act, lambda e: e.activation(out=out, in_=in_, func=func, **kw), reads=reads, writes=writes, pwrites=pwrites)

    def copy_any(self, out, in_, reads, writes=(), pwrites=()):
        self.rr += 1
        if self.rr % 2:
            self.op(self.act, lambda e: e.copy(out=out, in_=in_), reads=reads, writes=writes, pwrites=pwrites)
        else:
            self.op(self.dve, lambda e: e.tensor_copy(out=out, in_=in_), reads=reads, writes=writes, pwrites=pwrites)

    def tt(self, eng, out, in0, in1, op, reads, writes=(), pwrites=()):
        self.op(eng, lambda e: e.tensor_tensor(out=out, in0=in0, in1=in1, op=op), reads=reads, writes=writes, pwrites=pwrites)

    def ts(self, eng, out, in0, s1, s2, op0, op1=None, reads=(), writes=(), pwrites=()):
        if op1 is None:
            self.op(eng, lambda e: e.tensor_scalar(out=out, in0=in0, scalar1=s1, scalar2=None, op0=op0), reads=reads, writes=writes, pwrites=pwrites)
        else:
            self.op(eng, lambda e: e.tensor_scalar(out=out, in0=in0, scalar1=s1, scalar2=s2, op0=op0, op1=op1), reads=reads, writes=writes, pwrites=pwrites)

    def rstd(self, out, in_, scale, buf):
        self.op(self.act, lambda e: e.activation(out=out, in_=in_, func=AF.Sqrt, scale=scale, bias=self.eps_ap), reads=[buf], pwrites=[buf])
        self.op(self.dve, lambda e: e.reciprocal(out=out, in_=out), reads=[buf], pwrites=[buf])

    def stt(self, eng, out, in0, scalar, in1, op0, op1, reads, writes=(), pwrites=()):
        self.op(eng, lambda e: e.scalar_tensor_tensor(out=out, in0=in0, scalar=scalar, in1=in1, op0=op0, op1=op1),
                reads=reads, writes=writes, pwrites=pwrites)


def bcast_rows(ap_row, n=128):
    a = ap_row.partition_broadcast(n)
    if len(a.shape) == 3:
        a = a.rearrange("p o f -> p (o f)")
    return a


CO_ID, CO_ONES, CO_TRIF, CO_TRIB, CO_RM, CO_NEGF, CO_NEGB, CO_NSEL = 0, 128, 256, 384, 512, 640, 768, 896
NCONST = 896 + 2048


def make_consts():
    c = np.zeros((128, NCONST), np.float32)
    i = np.arange(128)
    c[:, CO_ID:CO_ID + 128] = np.eye(128)
    c[:, CO_ONES:CO_ONES + 128] = 1.0
    c[:, CO_TRIF:CO_TRIF + 128] = (i[:, None] <= i[None, :])
    c[:, CO_TRIB:CO_TRIB + 128] = (i[:, None] >= i[None, :])
    r = np.zeros((128, 128), np.float32)
    r[2 * np.arange(64), 2 * np.arange(64) + 1] = 1.0
    r[2 * np.arange(64) + 1, 2 * np.arange(64)] = -1.0
    c[:, CO_RM:CO_RM + 128] = r
    c[:, CO_NEGF:CO_NEGF + 128] = np.where(i[:, None] <= i[None, :], 0.0, -30000.0)
    c[:, CO_NEGB:CO_NEGB + 128] = np.where(i[:, None] >= i[None, :], 0.0, -30000.0)
    ns = np.zeros((16, 16, 128), np.float32)
    ns[np.arange(16), np.arange(16), :] = 1.0
    c[0:16, CO_NSEL:] = ns.reshape(16, 2048)
    c[32:48, CO_NSEL:] = ns.reshape(16, 2048)
    return c


def make_rope():
    n_freq = 32
    inv = (10000.0 ** (-np.arange(n_freq, dtype=np.float32) / n_freq)).astype(np.float32)
    t = np.arange(SEQ)
    row = (t // 64).astype(np.float32)
    col = (t % 64).astype(np.float32)
    ang = np.concatenate([row[:, None] * inv, col[:, None] * inv], -1).astype(np.float32)
    cos = np.cos(ang).astype(np.float32)
    sin = np.sin(ang).astype(np.float32)
    cosT = np.repeat(cos.T, 2, axis=0)
    sinT = np.repeat(sin.T, 2, axis=0)
    return np.ascontiguousarray(cosT), np.ascontiguousarray(sinT)


class Prog:
    def __init__(self, debug=()):
        self.debug = set(debug)
        nc = bass.Bass("TRN2", target_bir_lowering=False)
        self.nc = nc
        self.fw = FW(nc)

        def din(name, shape, dt=F32):
            return nc.dram_tensor(name, list(shape), dt, kind="ExternalInput").ap()

        def scr(name, shape, dt):
            if name in self.debug:
                t = nc.dram_tensor(name, list(shape), dt, kind="ExternalOutput").ap()
            else:
                t = nc.dram_tensor(name, list(shape), dt).ap()
            return t, Buf(name, self.fw)
        self.xin = din("xin", [2, SEQ, D])
        self.ctxin = din("ctxin", [2, CTX, D])
        self.cst = din("cst", [128, 16, 3])
        self.w_mod = din("w_mod", [2, D, 3 * D])
        self.w_in = din("w_in", [2, D, NIN])
        self.w_gate = din("w_gate", [2, D, 3 * D])
        self.w_branch = din("w_branch", [2, 3, 1024, D])
        self.w_out = din("w_out", [2, D, D])
        self.b_mod = din("b_mod", [2, 3 * D])
        self.g_pre = din("g_pre", [2, D])
        self.g_post = din("g_post", [2, D])
        self.b_gateT = din("b_gateT", [2, 128, 48])
        self.att_sink = din("att_sink", [2, 8])
        self.ml_gb = din("ml_gb", [2, 16])
        self.ml_norm = din("ml_norm", [2, 1024])
        self.conv_wT = din("conv_wT", [2, 128, 12, 5])
        self.conv_bT = din("conv_bT", [2, 128, 12])
        self.dt_bias = din("dt_bias", [2, 32])
        self.a_log = din("a_log", [2, 32])
        self.ssm_d = din("ssm_d", [2, 16])
        self.ssm_norm = din("ssm_norm", [2, 1024])
        self.consts = din("consts", [128, NCONST])
        self.cosT = din("cosT", [128, SEQ])
        self.sinT = din("sinT", [128, SEQ])
        self.out = nc.dram_tensor("out", [2, SEQ, D], F32, kind="ExternalOutput").ap()
        self.b_out = Buf("out", self.fw)
        self.mods, self.b_mods = scr("mods", [2, 3, 3 * D], F32)
        self.xs1, self.b_xs1 = scr("xs1", [2, T, D], F32)
        self.hTs, self.b_hTs = scr("hTs", [16, 128, T], BF16)
        self.QTs, self.b_QTs = scr("QTs", [8, 128, T], BF16)
        self.KTs, self.b_KTs = scr("KTs", [2, 128, T], BF16)
        self.Vs, self.b_Vs = scr("Vs", [T, 256], BF16)
        self.ZTs, self.b_ZTs = scr("ZTs", [8, 128, T], BF16)
        self.MQTs, self.b_MQTs = scr("MQTs", [8, 128, T], BF16)
        self.MKTs, self.b_MKTs = scr("MKTs", [8, 128, T], BF16)
        self.MKs, self.b_MKs = scr("MKs", [T, 1024], BF16)
        self.MVs, self.b_MVs = scr("MVs", [T, 1024], BF16)
        self.MOs, self.b_MOs = scr("MOs", [T, 1024], BF16)
        self.MZs, self.b_MZs = scr("MZs", [T, 1024], BF16)
        self.Gs, self.b_Gs = scr("Gs", [T, 16], F32)
        self.DTs, self.b_DTs = scr("DTs", [T, 32], F32)
        self.SXs, self.b_SXs = scr("SXs", [T, 1024], BF16)
        self.SBs, self.b_SBs = scr("SBs", [T, 256], BF16)
        self.SBTs, self.b_SBTs = scr("SBTs", [2, 128, T], BF16)
        self.SCTs, self.b_SCTs = scr("SCTs", [2, 128, T], BF16)
        self.SZs, self.b_SZs = scr("SZs", [T, 1024], BF16)
        self.OTs, self.b_OTs = scr("OTs", [3, 8, 128, T], BF16)
        self.HFs, self.b_HFs = scr("HFs", [T, 1024], F32)
        self.YFs, self.b_YFs = scr("YFs", [T, 1024], F32)
        self.rows, self.b_rows = scr("rows", [NT, 2, 2, 2048], BF16)
        self.wg16, self.b_wg16 = scr("wg16", [12, 128, 16 * 512], BF16)
        self.wb16, self.b_wb16 = scr("wb16", [12, 128, 8 * 512], BF16)
        self.wo16, self.b_wo16 = scr("wo16", [4, 128, 16 * 512], BF16)

    def load_consts(self, sc, names):
        fw = self.fw
        out = {}
        for key, (off, width, dt, rows) in names.items():
            t, b = sc.tile("c_" + key, [rows, width], dt)
            if dt == BF16:
                fw.dma(t[:], self.consts[0:rows, off:off + width], writes=[b], eng=fw.pool)
            else:
                fw.dma(t[:], self.consts[0:rows, off:off + width], writes=[b])
            out[key] = (t, b)
        return out

    def phase_mod(self):
        fw = self.fw
        with fw.scope() as sc:
            cs, b_cs = sc.tile("cs", [128, 16, 3], F32)
            scT, b_scT = sc.tile("scT", [128, 16, 3], BF16)
            fw.dma(cs[:], self.cst, writes=[b_cs])
            fw.acti(scT[:], cs[:], AF.Silu, reads=[b_cs], writes=[b_scT])
            wr = sc.ring("wm", 2, [128, 16, 512], BF16)
            pr = sc.ring("pm", 2, [128, 512], F32, psum=True)
            br = sc.ring("bm", 2, [3, 512], F32)
            mr = sc.ring("mm", 2, [3, 512], F32)
            for l in range(2):
                for cb in range(12):
                    w, b_w = wr.next()
                    fw.dma(w[:], self.w_mod[l, :, cb * 512:(cb + 1) * 512].rearrange("(k p) c -> p k c", p=128),
                           writes=[b_w], eng=fw.pool)
                    bm, b_bm = br.next()
                    fw.dma(bm[:], bcast_rows(self.b_mod[l:l + 1, cb * 512:(cb + 1) * 512], 3), writes=[b_bm])
                    ps, b_ps = pr.next()
                    fw.mm(ps[0:3, :], [(scT[:, k, :], w[:, k, :]) for k in range(16)], [b_scT, b_w], b_ps)
                    m, b_m = mr.next()
                    fw.tt(fw.dve, m[:], ps[0:3, :], bm[:], ALU.add, reads=[b_ps, b_bm], writes=[b_m])
                    fw.dma(self.mods[l, :, cb * 512:(cb + 1) * 512], m[:], reads=[b_m], pwrites=[self.b_mods])

    def src_tile(self, l, s, i):
        if l == 0:
            if i < 2:
                return self.ctxin[s, i * 128:(i + 1) * 128, :]
            return self.xin[s, (i - 2) * 128:(i - 1) * 128, :]
        return self.xs1[s, i * 128:(i + 1) * 128, :]

    def phase_norm(self, sc, l, s, hT, b_hT, cn):
        fw = self.fw
        ident, b_id = cn["ident"]
        with fw.scope() as s2:
            rows = {}
            for j, nm in ((s, "x"), (2, "c")):
                s1, b_s1 = s2.tile("s1" + nm, [128, D], F32)
                sh, b_sh = s2.tile("sh" + nm, [128, D], F32)
                rows[nm] = (s1, b_s1, sh, b_sh)
            with fw.scope() as s3:
                gpre, b_gpre = s3.tile("gpre", [128, D], F32)
                fw.dma(gpre[:], bcast_rows(self.g_pre[l:l + 1, :]), writes=[b_gpre])
                tmp, b_tmp = s3.tile("sctmp", [128, D], F32)
                for j, nm in ((s, "x"), (2, "c")):
                    s1, b_s1, sh, b_sh = rows[nm]
                    fw.dma(tmp[:], bcast_rows(self.mods[l, j:j + 1, D:2 * D]), reads=[self.b_mods], writes=[b_tmp])
                    fw.dma(sh[:], bcast_rows(self.mods[l, j:j + 1, 0:D]), reads=[self.b_mods], writes=[b_sh])
                    fw.stt(fw.dve, s1[:], tmp[:], 1.0, gpre[:], ALU.add, ALU.mult, reads=[b_tmp, b_gpre], writes=[b_s1])
            xr = s2.ring("xt", 3, [128, D], F32)
            jr = s2.ring("junk", 1, [128, D], BF16)
            ssr = s2.ring("ss", 4, [128, 2], F32)
            hnr = s2.ring("hn", 3, [128, D], F32)
            hbr = s2.ring("hb", 3, [128, D], BF16)
            ptr = s2.ring("ptr", 2, [128, 16, 128], BF16, psum=True)
            for i in range(NT):
                s1, b_s1, sh, b_sh = rows["c" if i < 2 else "x"]
                xt, b_xt = xr.next()
                src_b = [self.b_xs1] if l > 0 else []
                fw.dma(xt[:], self.src_tile(l, s, i), reads=src_b, writes=[b_xt])
                jk, b_jk = jr.next()
                ss, b_ss = ssr.next()
                fw.acti(jk[:], xt[:], AF.Square, reads=[b_xt], writes=[b_jk, b_ss], accum_out=ss[:, 0:1])
                fw.rstd(ss[:, 1:2], ss[:, 0:1], 1.0 / D, b_ss)
                hn, b_hn = hnr.next()
                fw.stt(fw.dve, hn[:], xt[:], ss[:, 1:2], s1[:], ALU.mult, ALU.mult, reads=[b_xt, b_ss, b_s1], writes=[b_hn])
                hb, b_hb = hbr.next()
                fw.tt(fw.pool, hb[:], hn[:], sh[:], ALU.add, reads=[b_hn, b_sh], writes=[b_hb])
                pt, b_pt = ptr.next()
                for k in range(16):
                    fw.tr(pt[:, k, :], hb[:, k * 128:(k + 1) * 128], ident[:], [b_hb, b_id], b_pt, last=(k == 15), pw=(k > 0))
                fw.copy_any(hT[:, :, i * 128:(i + 1) * 128], pt[:], reads=[b_pt], pwrites=[b_hT])
        for k in range(16):
            fw.dma(self.hTs[k], hT[:, k, :], reads=[b_hT], pwrites=[self.b_hTs])

    def phase_proj(self, l, s):
        fw = self.fw
        with fw.scope() as sc:
            cn = self.load_consts(sc, {"ident": (CO_ID, 128, BF16, 128), "rmat": (CO_RM, 128, BF16, 128)})
            ident, b_id = cn["ident"]
            rmat, b_rm = cn["rmat"]
            hT, b_hT = sc.tile("hT", [128, 16, T], BF16)
            self.phase_norm(sc, l, s, hT, b_hT, cn)
            import os as _os
            CUT = _os.environ.get("CUT", "")
            if CUT == "norm":
                return
            groups = [(0, 256)] + [(256 + 512 * m, 512) for m in range(4)]
            wr = sc.ring("wblk", 3, [128, 16, 512], BF16)
            pr = sc.ring("pp", 3, [128, 512], F32, psum=True)

            def load_w(c0, ncols):
                w, b_w = wr.next()
                fw.dma(w[:, :, 0:ncols], self.w_in[l, :, c0:c0 + ncols].rearrange("(k p) c -> p k c", p=128),
                       writes=[b_w], eng=fw.pool)
                return w, b_w

            with fw.scope() as s2:
                cos, b_cos = s2.tile("cos", [128, SEQ], F32)
                sin, b_sin = s2.tile("sin", [128, SEQ], F32)
                fw.dma(cos[:], self.cosT, writes=[b_cos])
                fw.dma(sin[:], self.sinT, writes=[b_sin])
                sgr = s2.ring("stg", 3, [128, 512], BF16)
                qsr = s2.ring("qs", 2, [128, 512], BF16)
                t1r = s2.ring("t1", 2, [128, 512], F32)
                t2r = s2.ring("t2", 2, [128, 512], F32)
                p2r = s2.ring("pr", 2, [128, 512], F32, psum=True)
                fam = [("q", C_AQ, 8, self.QTs, self.b_QTs), ("q", C_AK, 2, self.KTs, self.b_KTs),
                       ("z", C_AZ, 8, self.ZTs, self.b_ZTs), ("c", C_MQ, 8, self.MQTs, self.b_MQTs),
                       ("c", C_MK, 8, self.MKTs, self.b_MKTs)]
                FAM = _os.environ.get("FAM", "")
                if FAM:
                    fam = [fam[int(c)] for c in FAM]
                for kind, c0, nch, dst, b_dst in fam:
                    for cb in range(0, nch, 4):
                        nb = min(4, nch - cb)
                        w, b_w = load_w(c0 + cb * 128, nb * 128)
                        for j in range(nb):
                            ch = cb + j
                            for (g0, gs) in groups:
                                ps, b_ps = pr.next()
                                fw.mm(ps[:, 0:gs], [(w[:, k, j * 128:(j + 1) * 128], hT[:, k, g0:g0 + gs]) for k in range(16)],
                                      [b_w, b_hT], b_ps)
                                st, b_st = sgr.next()
                                if kind == "z":
                                    fw.acti(st[:, 0:gs], ps[:, 0:gs], AF.Silu, reads=[b_ps], writes=[b_st])
                                elif kind == "c" or g0 < CTX:
                                    fw.copy_any(st[:, 0:gs], ps[:, 0:gs], reads=[b_ps], writes=[b_st])
                                else:
                                    x0 = g0 - CTX
                                    qs, b_qs = qsr.next()
                                    fw.op(fw.act, lambda e: e.copy(out=qs[:], in_=ps[:]), reads=[b_ps], writes=[b_qs])
                                    p2, b_p2 = p2r.next()
                                    fw.mm(p2[:], [(rmat[:], qs[:])], [b_rm, b_qs], b_p2)
                                    t1, b_t1 = t1r.next()
                                    t2, b_t2 = t2r.next()
                                    fw.tt(fw.dve, t1[:], ps[:], cos[:, x0:x0 + 512], ALU.mult, reads=[b_ps, b_cos], writes=[b_t1])
                                    fw.tt(fw.dve, t2[:], p2[:], sin[:, x0:x0 + 512], ALU.mult, reads=[b_p2, b_sin], writes=[b_t2])
                                    fw.tt(fw.pool, st[:], t1[:], t2[:], ALU.add, reads=[b_t1, b_t2], writes=[b_st])
                                fw.dma(dst[ch, :, g0:g0 + gs], st[:, 0:gs], reads=[b_st], pwrites=[b_dst])

            if CUT == "fm":
                return
            with fw.scope() as s2:
                cw, b_cw = s2.tile("cw", [128, 12, 5], F32)
                cb_, b_cb = s2.tile("cb", [128, 12], F32)
                fw.dma(cw[:], self.conv_wT[l], writes=[b_cw])
                fw.dma(cb_[:], self.conv_bT[l], writes=[b_cb])
                W = T + 8
                stripr = s2.ring("strip", 2, [128, W], F32)
                for (st_, b_s) in stripr.items:
                    fw.op(fw.pool, lambda e, st_=st_: e.memset(st_[:], 0.0), writes=[b_s])
                accr = s2.ring("acc", 2, [128, T + 4], F32)
                slr = s2.ring("sl", 3, [128, T + 4], BF16)
                ptr = s2.ring("ptx", 2, [128, 8, 128], BF16, psum=True)
                stgr = s2.ring("xstg", 2, [128, NT, 128], BF16)

                def tokcol(i):
                    return i * 128 if i < 2 else 260 + (i - 2) * 128
                deferred = []
                for cb in range(0, 12, 4):
                    w, b_w = load_w(C_SX + cb * 128, 512)
                    for j in range(4):
                        ch = cb + j
                        strip, b_strip = stripr.next()
                        first = True
                        for (g0, gs) in groups:
                            ps, b_ps = pr.next()
                            fw.mm(ps[:, 0:gs], [(w[:, k, j * 128:(j + 1) * 128], hT[:, k, g0:g0 + gs]) for k in range(16)],
                                  [b_w, b_hT], b_ps)
                            c0 = 2 + g0 if g0 < CTX else 262 + (g0 - CTX)
                            if first:
                                fw.copy_any(strip[:, c0:c0 + gs], ps[:, 0:gs], reads=[b_ps], writes=[b_strip])
                                first = False
                            else:
                                fw.copy_any(strip[:, c0:c0 + gs], ps[:, 0:gs], reads=[b_ps], pwrites=[b_strip])
                        while deferred:
                            deferred.pop(0)()
                        acc, b_acc = accr.next()
                        n = T + 4
                        fw.acti(acc[:], strip[:, 0:n], AF.Identity, reads=[b_strip, b_cw, b_cb], writes=[b_acc],
                                scale=cw[:, ch, 0:1], bias=cb_[:, ch:ch + 1])
                        for kk in range(1, 5):
                            fw.stt(fw.dve, acc[:], strip[:, kk:kk + n], cw[:, ch, kk:kk + 1], acc[:], ALU.mult, ALU.add,
                                   reads=[b_strip, b_cw, b_acc], pwrites=[b_acc])
                        sl, b_sl = slr.next()
                        fw.acti(sl[:], acc[:], AF.Silu, reads=[b_acc], writes=[b_sl])
                        if ch >= 8:
                            dst, b_dst = (self.SBTs, self.b_SBTs) if ch < 10 else (self.SCTs, self.b_SCTs)
                            g = (ch - 8) % 2
                            fw.dma(dst[g, :, 0:CTX], sl[:, 0:CTX], reads=[b_sl], pwrites=[b_dst])
                            fw.dma(dst[g, :, CTX:T], sl[:, 260:260 + SEQ], reads=[b_sl], pwrites=[b_dst])
                        if ch < 10:
                            def emit_tr(ch=ch, sl=sl, b_sl=b_sl):
                                stg, b_stg = stgr.next()
                                for i0 in range(0, NT, 8):
                                    nn = min(8, NT - i0)
                                    pt, b_pt = ptr.next()
                                    for ii in range(nn):
                                        c = tokcol(i0 + ii)
                                        fw.tr(pt[:, ii, :], sl[:, c:c + 128], ident[:], [b_sl, b_id], b_pt, last=(ii == nn - 1), pw=(ii > 0))
                                    if i0 == 0:
                                        fw.copy_any(stg[:, i0:i0 + nn, :], pt[:, 0:nn, :], reads=[b_pt], writes=[b_stg])
                                    else:
                                        fw.copy_any(stg[:, i0:i0 + nn, :], pt[:, 0:nn, :], reads=[b_pt], pwrites=[b_stg])
                                if ch < 8:
                                    fw.dma(self.SXs[:, ch * 128:(ch + 1) * 128].rearrange("(i p) c -> p i c", p=128), stg[:],
                                           reads=[b_stg], pwrites=[self.b_SXs])
                                else:
                                    fw.dma(self.SBs[:, (ch - 8) * 128:(ch - 7) * 128].rearrange("(i p) c -> p i c", p=128), stg[:],
                                           reads=[b_stg], pwrites=[self.b_SBs])
                            deferred.append(emit_tr)
                while deferred:
                    deferred.pop(0)()

            if CUT == "xbc":
                return
            with fw.scope() as s2:
                sgr = s2.ring("tstg", 3, [128, 512], BF16)
                sfr = s2.ring("fstg", 2, [128, 32], F32)
                fam = [("c", C_AV, 256, self.Vs, self.b_Vs), ("c", C_MK, 1024, self.MKs, self.b_MKs),
                       ("c", C_MV, 1024, self.MVs, self.b_MVs), ("sig", C_MO, 1024, self.MOs, self.b_MOs),
                       ("silu", C_MZ, 1024, self.MZs, self.b_MZs), ("f", C_MG, 16, self.Gs, self.b_Gs),
                       ("f", C_SDT, 32, self.DTs, self.b_DTs), ("silu", C_SZ, 1024, self.SZs, self.b_SZs)]
                for kind, c0, ncols, dst, b_dst in fam:
                    for cb in range(0, ncols, 512):
                        nb = min(512, ncols - cb)
                        w, b_w = load_w(c0 + cb, nb)
                        for i in range(NT):
                            ps, b_ps = pr.next()
                            fw.mm(ps[:, 0:nb], [(hT[:, k, i * 128:(i + 1) * 128], w[:, k, 0:nb]) for k in range(16)],
                                  [b_w, b_hT], b_ps)
                            if kind == "f":
                                st, b_st = sfr.next()
                                fw.copy_any(st[:, 0:nb], ps[:, 0:nb], reads=[b_ps], writes=[b_st])
                            else:
                                st, b_st = sgr.next()
                                if kind == "c":
                                    fw.copy_any(st[:, 0:nb], ps[:, 0:nb], reads=[b_ps], writes=[b_st])
                                else:
                                    fw.acti(st[:, 0:nb], ps[:, 0:nb], AF.Sigmoid if kind == "sig" else AF.Silu,
                                            reads=[b_ps], writes=[b_st])
                            fw.dma(dst[i * 128:(i + 1) * 128, cb:cb + nb], st[:, 0:nb], reads=[b_st], pwrites=[b_dst])

    def phase_att(self, l, s):
        fw = self.fw
        scale = 128.0 ** -0.5
        with fw.scope() as sc:
            cn = self.load_consts(sc, {"ones": (CO_ONES, 128, BF16, 128), "triF": (CO_TRIF, 128, BF16, 128),
                                       "triB": (CO_TRIB, 128, BF16, 128)})
            ones, b_ones = cn["ones"]
            KT, b_KT = sc.tile("KT", [128, 2, T], BF16)
            V, b_V = sc.tile("V", [128, NT, 256], BF16)
            fw.dma(KT[:], self.KTs.rearrange("g p t -> p g t"), reads=[self.b_KTs], writes=[b_KT])
            fw.dma(V[:], self.Vs.rearrange("(i p) c -> p i c", p=128), reads=[self.b_Vs], writes=[b_V])
            snk, b_snk = sc.tile("snk", [128, 8], F32)
            esk, b_esk = sc.tile("esk", [128, 8, 128], F32)
            fw.dma(snk[:], bcast_rows(self.att_sink[l:l + 1, :]), writes=[b_snk])
            fw.acti(snk[:], snk[:], AF.Exp, reads=[b_snk], writes=[b_snk])
            fw.op(fw.dve, lambda e: e.tensor_copy(out=esk[:], in_=snk[:].unsqueeze(2).to_broadcast([128, 8, 128])),
                  reads=[b_snk], writes=[b_esk])
            qr = sc.ring("qT", 2, [128, 8, 128], BF16)
            zr = sc.ring("zT", 2, [128, 8, 128], BF16)
            osr = sc.ring("ost", 2, [128, 8, 128], BF16)
            psr = sc.ring("pss", 3, [128, 512], F32, psum=True)
            por = sc.ring("pso", 2, [128, 512], F32, psum=True)
            pdr = sc.ring("psd", 2, [128, 512], F32, psum=True)
            ptr_ = sc.ring("pT", 6, [128, 512], BF16)
            ddr = sc.ring("dd", 2, [128, 512], F32)
            tor = sc.ring("to", 2, [128, 512], F32)
            tiles = list(range(2, NT)) + ([0, 1] if l == 0 else [])
            for ti in tiles:
                if ti >= 2:
                    keys = []
                    if ti > 2:
                        keys.append((ti - 1, "triB"))
                    keys.append((ti, None))
                    if ti < NT - 1:
                        keys.append((ti + 1, "triF"))
                    keys += [(0, None), (1, None)]
                else:
                    keys = [(0, None), (1, None)]
                qT, b_qT = qr.next()
                zT, b_zT = zr.next()
                fw.dma(qT[:], self.QTs[:, :, ti * 128:(ti + 1) * 128].rearrange("h p t -> p h t"), reads=[self.b_QTs], writes=[b_qT])
                fw.dma(zT[:], self.ZTs[:, :, ti * 128:(ti + 1) * 128].rearrange("h p t -> p h t"), reads=[self.b_ZTs], writes=[b_zT])
                ost, b_ost = osr.next()
                for g in range(2):
                    q2 = qT[:, 4 * g:4 * g + 4, :].rearrange("p a b -> p (a b)")
                    pts = []
                    for (kt, msk) in keys:
                        ps, b_ps = psr.next()
                        fw.mm(ps[:], [(KT[:, g, kt * 128:(kt + 1) * 128], q2)], [b_KT, b_qT], b_ps)
                        pT, b_pT = ptr_.next()
                        fw.acti(pT[:], ps[:], AF.Exp, reads=[b_ps], writes=[b_pT], scale=scale)
                        if msk is not None:
                            m, b_m = cn[msk]
                            p3 = pT[:].rearrange("p (a b) -> p a b", a=4)
                            fw.tt(fw.dve, p3, p3, m[:].unsqueeze(1).to_broadcast([128, 4, 128]), ALU.mult,
                                  reads=[b_pT, b_m], writes=[b_pT])
                        pts.append((kt, pT, b_pT))
                    po, b_po = por.next()
                    pd, b_pd = pdr.next()
                    fw.mm(po[:], [(V[:, kt, g * 128:(g + 1) * 128], pT[:]) for (kt, pT, _) in pts], [b_V] + [b for (_, _, b) in pts], b_po)
                    fw.mm(pd[:], [(ones[:], pT[:]) for (kt, pT, _) in pts], [b_ones] + [b for (_, _, b) in pts], b_pd)
                    dd, b_dd = ddr.next()
                    fw.tt(fw.dve, dd[:], pd[:], esk[:, 4 * g:4 * g + 4, :].rearrange("p a b -> p (a b)"), ALU.add,
                          reads=[b_pd, b_esk], writes=[b_dd])
                    fw.op(fw.dve, lambda e: e.reciprocal(out=dd[:], in_=dd[:]), reads=[b_dd], writes=[b_dd])
                    to, b_to = tor.next()
                    fw.tt(fw.dve, to[:], po[:], dd[:], ALU.mult, reads=[b_po, b_dd], writes=[b_to])
                    fw.tt(fw.pool, ost[:, 4 * g:4 * g + 4, :].rearrange("p a b -> p (a b)"), to[:],
                          zT[:, 4 * g:4 * g + 4, :].rearrange("p a b -> p (a b)"), ALU.mult,
                          reads=[b_to, b_zT], writes=[b_ost] if g == 0 else (), pwrites=() if g == 0 else [b_ost])
                fw.dma(self.OTs[0, :, :, ti * 128:(ti + 1) * 128].rearrange("h p t -> p h t"), ost[:], reads=[b_ost], pwrites=[self.b_OTs])

    def phase_mlstm(self, l, s):
        fw = self.fw
        with fw.scope() as sc:
            cn = self.load_consts(sc, {"ident": (CO_ID, 128, BF16, 128), "triFb": (CO_TRIF, 128, BF16, 128),
                                       "triBb": (CO_TRIB, 128, BF16, 128)})
            ident, b_id = cn["ident"]
            r_ = {}; c_ = {}; wd_ = {}; gd_ = {}
            bufs_g = []
            for d in range(2):
                for nm, dct in (("r", r_), ("c", c_), ("wd", wd_), ("gd", gd_)):
                    t, b = sc.tile(f"g{nm}{d}", [128, NT, 4], F32)
                    dct[d] = (t, b)
            with fw.scope() as s2:
                c2 = self.load_consts(s2, {"triF": (CO_TRIF, 128, F32, 128), "triB": (CO_TRIB, 128, F32, 128),
                                           "ones": (CO_ONES, 128, F32, 128)})
                Gt, b_Gt = s2.tile("Gt", [128, NT, 16], F32)
                gbias, b_gbias = s2.tile("gbias", [128, 16], F32)
                fw.dma(Gt[:], self.Gs.rearrange("(i p) c -> p i c", p=128), reads=[self.b_Gs], writes=[b_Gt])
                fw.dma(gbias[:], bcast_rows(self.ml_gb[l:l + 1, :]), writes=[b_gbias])
                fw.tt(fw.dve, Gt[:], Gt[:], gbias[:].unsqueeze(1).to_broadcast([128, NT, 16]), ALU.add,
                      reads=[b_Gt, b_gbias], writes=[b_Gt])
                for d in range(2):
                    ig = Gt[:, :, 8 * d:8 * d + 4]
                    fg = Gt[:, :, 8 * d + 4:8 * d + 8]
                    sp, b_sp = s2.tile(f"sp{d}", [128, NT, 4], F32)
                    a, b_a = s2.tile(f"a{d}", [128, NT, 4], F32)
                    fw.acti(sp[:], fg, AF.Exp, reads=[b_Gt], writes=[b_sp], scale=-1.0)
                    fw.acti(sp[:], sp[:], AF.Ln, reads=[b_sp], writes=[b_sp], bias=fw.one_ap)
                    pb, b_pb = s2.ptile(f"pb{d}", [128, 512], F32)
                    pt, b_pt = s2.ptile(f"ptot{d}", [128, 512], F32)
                    tri, b_tri = c2["triF" if d == 0 else "triB"]
                    on, b_on = c2["ones"]
                    sp2 = sp[:].rearrange("p a b -> p (a b)")
                    fw.mm(pb[:, 0:NT * 4], [(tri[:], sp2)], [b_tri, b_sp], b_pb)
                    fw.mm(pt[:, 0:NT * 4], [(on[:], sp2)], [b_on, b_sp], b_pt)
                    pb3 = pb[:, 0:NT * 4].rearrange("p (a b) -> p a b", b=4)
                    pt3 = pt[:, 0:NT * 4].rearrange("p (a b) -> p a b", b=4)
                    fw.acti(r_[d][0][:], pb3, AF.Exp, reads=[b_pb], writes=[r_[d][1]], scale=-1.0)
                    fw.acti(gd_[d][0][:], pt3, AF.Exp, reads=[b_pt], writes=[gd_[d][1]], scale=-1.0)
                    fw.tt(fw.dve, a[:], ig, pb3, ALU.add, reads=[b_Gt, b_pb], writes=[b_a])
                    fw.acti(c_[d][0][:], a[:], AF.Exp, reads=[b_a], writes=[c_[d][1]], bias=fw.nln16_ap)
                    fw.tt(fw.dve, a[:], a[:], pt3, ALU.subtract, reads=[b_a, b_pt], writes=[b_a])
                    fw.acti(wd_[d][0][:], a[:], AF.Exp, reads=[b_a], writes=[wd_[d][1]], bias=fw.nln16_ap)
            CstT = {}; CbfT = {}
            for d in range(2):
                for h in range(4):
                    CstT[d, h] = sc.tile(f"Cst{d}{h}", [128, 2, 257], F32)
                    CbfT[d, h] = sc.tile(f"Cbf{d}{h}", [128, 2, 257], BF16)
                    fw.op(fw.pool, lambda e, t=CstT[d, h][0]: e.memset(t[:], 0.0), writes=[CstT[d, h][1]])
                    fw.op(fw.pool, lambda e, t=CbfT[d, h][0]: e.memset(t[:], 0.0), writes=[CbfT[d, h][1]])
            nrow, b_nrow = sc.tile("nrow", [128, 1024], F32)
            fw.dma(nrow[:], bcast_rows(self.ml_norm[l:l + 1, :]), writes=[b_nrow])
            qr = sc.ring("mq", 4, [128, 8, 128], BF16)
            kr = sc.ring("mkT", 4, [128, 8, 128], BF16)
            k2r = sc.ring("mk", 4, [128, 1024], BF16)
            var = sc.ring("vaug", 4, [128, 4, 257], BF16)
            for (va, b_va) in var.items:
                fw.op(fw.pool, lambda e, va=va: e.memset(va[:], 1.0), writes=[b_va])
            hdr = sc.ring("hdir", 4, [128, 1024], F32)
            PTr = sc.ring("PT", 4, [128, 128], BF16)
            kwr = sc.ring("kw", 4, [128, 256], BF16)
            fr = sc.ring("fac", 4, [128, 2], F32)
            pss = sc.ring("ms", 2, [128, 512], F32, psum=True)
            psn = sc.ring("mn", 2, [128, 512], F32, psum=True)
            psc = sc.ring("mc", 2, [128, 512], F32, psum=True)
            ptr = sc.ring("mtr", 2, [128, 8, 128], BF16, psum=True)
            hfr = sc.ring("hf", 3, [128, 1024], F32)
            mor = sc.ring("mo", 3, [128, 1024], BF16)
            mzr = sc.ring("mz", 3, [128, 1024], BF16)
            hsr = sc.ring("hs", 2, [128, 1024], F32)
            nzr = sc.ring("nz", 2, [128, 1024], F32)
            jr = sc.ring("mj", 2, [128, 256], BF16)
            ssr = sc.ring("mss", 4, [128, 8], F32)
            omr = sc.ring("om", 4, [128, 1024], BF16)
            stg = sc.ring("mstg", 4, [128, 8, 128], BF16)

            def head(d, ti, h, qT, b_qT, kT, b_kT, k, b_k, va, b_va, hd, b_hd, mask, b_mask):
                Cst, b_Cst = CstT[d, h]
                Cbf, b_Cbf = CbfT[d, h]
                ps, b_ps = pss.next()
                fw.mm(ps[:, 0:128], [(kT[:, 2 * h + j, :], qT[:, 2 * h + j, :]) for j in range(2)], [b_kT, b_qT], b_ps)
                kw, b_kw = kwr.next()
                fw.acti(kw[:], k[:, h * 256:(h + 1) * 256], AF.Copy, reads=[b_k, wd_[d][1]], writes=[b_kw],
                        scale=wd_[d][0][:, ti, h:h + 1])
                yield
                PT, b_PT = PTr.next()
                fw.stt(fw.dve, PT[:], ps[:, 0:128], c_[d][0][:, ti, h:h + 1], mask[:], ALU.mult, ALU.mult,
                       reads=[b_ps, c_[d][1], b_mask], writes=[b_PT])
                yield
                pn, b_pn = psn.next()
                fw.mm(pn[:, 0:257], [(qT[:, 2 * h, :], Cbf[:, 0, :]), (qT[:, 2 * h + 1, :], Cbf[:, 1, :]),
                                     (PT[:], va[:, h, :])], [b_qT, b_Cbf, b_PT, b_va], b_pn)
                for j in range(2):
                    pc, b_pc = psc.next()
                    fw.mm(pc[:, 0:257], [(kw[:, j * 128:(j + 1) * 128], va[:, h, :])], [b_kw, b_va], b_pc)
                    fw.stt(fw.dve, Cst[:, j, :], Cst[:, j, :], gd_[d][0][:, ti, h:h + 1], pc[:, 0:257],
                           ALU.mult, ALU.add, reads=[b_pc, gd_[d][1], b_Cst], pwrites=[b_Cst])
                yield
                f, b_f = fr.next()
                rr = r_[d][0][:, ti, h:h + 1]
                fw.acti(f[:, 0:1], pn[:, 256:257], AF.Abs, reads=[b_pn, r_[d][1]], writes=[b_f], scale=rr)
                fw.op(fw.act, lambda e: e.copy(out=Cbf[:], in_=Cst[:]), reads=[b_Cst], writes=[b_Cbf])
                yield
                fw.ts(fw.dve, f[:, 0:1], f[:, 0:1], 1.0, None, ALU.max, reads=[b_f], writes=[b_f])
                fw.op(fw.dve, lambda e: e.reciprocal(out=f[:, 0:1], in_=f[:, 0:1]), reads=[b_f], writes=[b_f])
                fw.ts(fw.dve, f[:, 1:2], f[:, 0:1], rr, None, ALU.mult, reads=[b_f, r_[d][1]], writes=[b_f])
                yield
                fw.acti(hd[:, h * 256:(h + 1) * 256], pn[:, 0:256], AF.Copy, reads=[b_pn, b_f],
                        writes=[b_hd] if h == 0 else (), pwrites=() if h == 0 else [b_hd], scale=f[:, 1:2])

            def lockstep(gens):
                gens = list(gens)
                while gens:
                    for g in list(gens):
                        try:
                            next(g)
                        except StopIteration:
                            gens.remove(g)
                    yield

            def step(d, ti, second):
                mask, b_mask = cn["triFb" if d == 0 else "triBb"]
                qT, b_qT = qr.next(); kT, b_kT = kr.next(); k, b_k = k2r.next(); va, b_va = var.next()
                tsl = slice(ti * 128, (ti + 1) * 128)
                fw.dma(qT[:], self.MQTs[:, :, tsl].rearrange("h p t -> p h t"), reads=[self.b_MQTs], writes=[b_qT])
                fw.dma(kT[:], self.MKTs[:, :, tsl].rearrange("h p t -> p h t"), reads=[self.b_MKTs], writes=[b_kT])
                fw.dma(k[:], self.MKs[tsl, :], reads=[self.b_MKs], writes=[b_k])
                fw.dma(va[:, :, 0:256], self.MVs[tsl, :].rearrange("p (h e) -> p h e", h=4), reads=[self.b_MVs], writes=[b_va])
                fin = second and not (l == 1 and ti < 2)
                if fin:
                    hf, b_hf = hfr.next(); mo, b_mo = mor.next(); mz, b_mz = mzr.next()
                    fw.dma(hf[:], self.HFs[tsl, :], reads=[self.b_HFs], writes=[b_hf])
                    fw.dma(mo[:], self.MOs[tsl, :], reads=[self.b_MOs], writes=[b_mo])
                    fw.dma(mz[:], self.MZs[tsl, :], reads=[self.b_MZs], writes=[b_mz])
                hd, b_hd = hdr.next()
                yield
                for h in range(4):
                    yield from head(d, ti, h, qT, b_qT, kT, b_kT, k, b_k, va, b_va, hd, b_hd, mask, b_mask)
                    yield
                if l == 1 and ti < 2:
                    return
                if not second:
                    fw.dma(self.HFs[tsl, :], hd[:], reads=[b_hd], pwrites=[self.b_HFs])
                    return
                hs, b_hs = hsr.next(); nz, b_nz = nzr.next()
                fw.tt(fw.dve, hs[:], hf[:], hd[:], ALU.add, reads=[b_hf, b_hd], writes=[b_hs])
                fw.tt(fw.pool, nz[:], mz[:], nrow[:], ALU.mult, reads=[b_mz, b_nrow], writes=[b_nz])
                yield
                fw.tt(fw.dve, hs[:], hs[:], mo[:], ALU.mult, reads=[b_hs, b_mo], writes=[b_hs])
                yield
                ss, b_ss = ssr.next()
                jk, b_jk = jr.next()
                for h in range(4):
                    fw.acti(jk[:], hs[:, h * 256:(h + 1) * 256], AF.Square, reads=[b_hs], writes=[b_jk],
                            pwrites=[b_ss], accum_out=ss[:, h:h + 1])
                yield
                fw.rstd(ss[:, 4:8], ss[:, 0:4], 1.0 / 256, b_ss)
                yield
                om, b_om = omr.next()
                for h in range(4):
                    hsl = slice(h * 256, (h + 1) * 256)
                    fw.stt(fw.dve, om[:, hsl], hs[:, hsl], ss[:, 4 + h:5 + h], nz[:, hsl], ALU.mult, ALU.mult,
                           reads=[b_hs, b_ss, b_nz], writes=[b_om] if h == 0 else (), pwrites=() if h == 0 else [b_om])
                yield
                pt, b_pt = ptr.next()
                for c in range(8):
                    fw.tr(pt[:, c, :], om[:, c * 128:(c + 1) * 128], ident[:], [b_om, b_id], b_pt, last=(c == 7), pw=(c > 0))
                yield
                st, b_st = stg.next()
                fw.copy_any(st[:], pt[:], reads=[b_pt], writes=[b_st])
                fw.dma(self.OTs[1, :, :, tsl].rearrange("h p t -> p h t"), st[:], reads=[b_st], pwrites=[self.b_OTs])

            order = (list(range(NT)), [1, 0] + list(range(NT - 1, 1, -1)))
            seen = set()
            for n in range(NT):
                gens = []
                for d in range(2):
                    ti = order[d][n]
                    gens.append(step(d, ti, ti in seen))
                    seen.add(ti)
                for _ in lockstep(gens):
                    pass

    def phase_ssd(self, l, s):
        fw = self.fw
        with fw.scope() as sc:
            cn = self.load_consts(sc, {"ident": (CO_ID, 128, BF16, 128), "psel": (CO_NSEL, 2048, BF16, 48),
                                       "negF": (CO_NEGF, 128, BF16, 128), "negB": (CO_NEGB, 128, BF16, 128),
                                       "triF": (CO_TRIF, 128, F32, 128), "triB": (CO_TRIB, 128, F32, 128)})
            ident, b_id = cn["ident"]
            psel, b_psel = cn["psel"]
            neg4 = {}
            for d, nm in ((0, "negF"), (1, "negB")):
                t, b = sc.tile("neg4" + nm, [128, 4, 128], BF16)
                fw.op(fw.dve, lambda e, t=t, nm=nm: e.tensor_copy(out=t[:], in_=cn[nm][0][:].unsqueeze(1).to_broadcast([128, 4, 128])),
                      reads=[cn[nm][1]], writes=[b])
                neg4[d] = (t, b)
            nones, b_nones = sc.tile("nones", [2, 128], BF16)
            fw.op(fw.pool, lambda e: e.memset(nones[:], -1.0), writes=[b_nones])
            dt_ = {}; rc_ = {}; gd_ = {}; wd_ = {}; nla48 = {}
            for d in range(2):
                for nm, dct in (("dt", dt_), ("rc", rc_), ("gd", gd_), ("wd", wd_)):
                    dct[d] = sc.tile(f"s{nm}{d}", [128, NT, 16], F32)
                nla48[d] = sc.tile(f"nla48{d}", [128, NT, 48], F32)
            with fw.scope() as s2:
                c2 = self.load_consts(s2, {"ones": (CO_ONES, 128, F32, 128)})
                DTt, b_DTt = s2.tile("DTt", [128, NT, 32], F32)
                dtb, b_dtb = s2.tile("dtb", [128, 32], F32)
                ea, b_ea = s2.tile("ea", [128, 32], F32)
                fw.dma(DTt[:], self.DTs.rearrange("(i p) c -> p i c", p=128), reads=[self.b_DTs], writes=[b_DTt])
                fw.dma(dtb[:], bcast_rows(self.dt_bias[l:l + 1, :]), writes=[b_dtb])
                fw.dma(ea[:], bcast_rows(self.a_log[l:l + 1, :]), writes=[b_ea])
                fw.acti(ea[:], ea[:], AF.Exp, reads=[b_ea], writes=[b_ea])
                fw.tt(fw.dve, DTt[:], DTt[:], dtb[:].unsqueeze(1).to_broadcast([128, NT, 32]), ALU.add, reads=[b_DTt, b_dtb], writes=[b_DTt])
                fw.acti(DTt[:], DTt[:], AF.Exp, reads=[b_DTt], writes=[b_DTt])
                fw.acti(DTt[:], DTt[:], AF.Ln, reads=[b_DTt], writes=[b_DTt], bias=fw.one_ap)
                for d in range(2):
                    dt, b_dt = dt_[d]
                    fw.op(fw.dve, lambda e: e.tensor_copy(out=dt[:], in_=DTt[:, :, 16 * d:16 * d + 16]), reads=[b_DTt], writes=[b_dt])
                    nla, b_nla = s2.tile(f"nla{d}", [128, NT, 16], F32)
                    fw.tt(fw.dve, nla[:], dt[:], ea[:, 16 * d:16 * d + 16].unsqueeze(1).to_broadcast([128, NT, 16]), ALU.mult,
                          reads=[b_dt, b_ea], writes=[b_nla])
                    n48, b_n48 = nla48[d]
                    fw.op(fw.pool, lambda e: e.memset(n48[:], 0.0), writes=[b_n48])
                    fw.op(fw.pool, lambda e: e.tensor_copy(out=n48[:, :, 0:16], in_=nla[:]), reads=[b_nla], pwrites=[b_n48])
                    fw.op(fw.pool, lambda e: e.tensor_copy(out=n48[:, :, 32:48], in_=nla[:]), reads=[b_nla], pwrites=[b_n48])
                    pcu, b_pcu = s2.ptile(f"pcu{d}", [128, 512], F32)
                    pto, b_pto = s2.ptile(f"pto{d}", [128, 512], F32)
                    tri, b_tri = cn["triF" if d == 0 else "triB"]
                    on, b_on = c2["ones"]
                    n2 = nla[:].rearrange("p a b -> p (a b)")
                    fw.mm(pcu[:, 0:NT * 16], [(tri[:], n2)], [b_tri, b_nla], b_pcu)
                    fw.mm(pto[:, 0:NT * 16], [(on[:], n2)], [b_on, b_nla], b_pto)
                    pc3 = pcu[:, 0:NT * 16].rearrange("p (a b) -> p a b", b=16)
                    pt3 = pto[:, 0:NT * 16].rearrange("p (a b) -> p a b", b=16)
                    fw.acti(rc_[d][0][:], pc3, AF.Exp, reads=[b_pcu], writes=[rc_[d][1]], scale=-1.0)
                    fw.acti(gd_[d][0][:], pt3, AF.Exp, reads=[b_pto], writes=[gd_[d][1]], scale=-1.0)
                    tmp, b_tmp = s2.tile(f"stmp{d}", [128, NT, 16], F32)
                    fw.op(fw.dve, lambda e: e.tensor_copy(out=tmp[:], in_=pc3), reads=[b_pcu], writes=[b_tmp])
                    fw.tt(fw.dve, tmp[:], tmp[:], pt3, ALU.subtract, reads=[b_tmp, b_pto], writes=[b_tmp])
                    fw.acti(tmp[:], tmp[:], AF.Exp, reads=[b_tmp], writes=[b_tmp])
                    fw.tt(fw.dve, wd_[d][0][:], tmp[:], dt[:], ALU.mult, reads=[b_tmp, b_dt], writes=[wd_[d][1]])
            HstT = {}; HbfT = {}
            for d in range(2):
                HstT[d] = sc.tile(f"Hst{d}", [128, 1024], F32)
                HbfT[d] = sc.tile(f"Hbf{d}", [128, 1024], BF16)
                fw.op(fw.pool, lambda e, t=HstT[d][0]: e.memset(t[:], 0.0), writes=[HstT[d][1]])
                fw.op(fw.pool, lambda e, t=HbfT[d][0]: e.memset(t[:], 0.0), writes=[HbfT[d][1]])
            nrow, b_nrow = sc.tile("snrow", [128, 1024], F32)
            dsk, b_dsk = sc.tile("dsk", [128, 16], F32)
            fw.dma(nrow[:], bcast_rows(self.ssm_norm[l:l + 1, :]), writes=[b_nrow])
            fw.dma(dsk[:], bcast_rows(self.ssm_d[l:l + 1, :]), writes=[b_dsk])
            cTr = sc.ring("cT", 4, [48, 128], BF16)
            for (t, b) in cTr.items:
                fw.op(fw.pool, lambda e, t=t: e.memset(t[:], 0.0), writes=[b])
            h2r = sc.ring("hi2", 4, [48, 128], BF16)
            rwr = sc.ring("rw", 4, [2, 2048], BF16)
            xr = sc.ring("sx", 4, [128, 1024], BF16)
            Btr = sc.ring("sB", 4, [128, 256], BF16)
            BTr = sc.ring("sBT", 4, [128, 2, 128], BF16)
            CTr = sc.ring("sCT", 4, [128, 2, 128], BF16)
            CBr = sc.ring("CBs", 4, [128, 2, 128], BF16)
            Lr = sc.ring("Lb", 4, [128, 16, 128], BF16)
            MTr = sc.ring("MT", 4, [128, 16, 128], BF16)
            xdr = sc.ring("xdt", 4, [128, 1024], BF16)
            xwr = sc.ring("xw", 4, [128, 1024], BF16)
            ydr = sc.ring("ydir", 4, [128, 1024], F32)
            t1r = sc.ring("st1", 2, [128, 512], F32)
            psm = sc.ring("psm", 1, [128, 512], F32, psum=True)
            pse = sc.ring("pse", 2, [128, 512], F32, psum=True)
            pyr = sc.ring("psy", 1, [128, 2, 512], F32, psum=True)
            pir = sc.ring("psi", 1, [128, 2, 512], F32, psum=True)
            ptr = sc.ring("str", 1, [128, 8, 128], BF16, psum=True)
            yfr = sc.ring("yf", 4, [128, 1024], F32)
            szr = sc.ring("sz", 4, [128, 1024], BF16)
            ytr = sc.ring("yt", 2, [128, 1024], F32)
            jr = sc.ring("sj", 2, [128, 1024], BF16)
            ssr = sc.ring("sss", 4, [128, 2], F32)
            osr = sc.ring("os", 4, [128, 1024], BF16)
            stg = sc.ring("sstg", 4, [128, 8, 128], BF16)

            def step(d, ti, second):
                tri, b_tri = cn["triF" if d == 0 else "triB"]
                n4, b_n4 = neg4[d]
                Hst, b_Hst = HstT[d]
                Hbf, b_Hbf = HbfT[d]
                tsl = slice(ti * 128, (ti + 1) * 128)
                x, b_x = xr.next(); Bt, b_Bt = Btr.next(); BT, b_BT = BTr.next(); CT, b_CT = CTr.next()
                fw.dma(x[:], self.SXs[tsl, :], reads=[self.b_SXs], writes=[b_x])
                fw.dma(Bt[:], self.SBs[tsl, :], reads=[self.b_SBs], writes=[b_Bt])
                fw.dma(BT[:], self.SBTs[:, :, tsl].rearrange("g p t -> p g t"), reads=[self.b_SBTs], writes=[b_BT])
                fw.dma(CT[:], self.SCTs[:, :, tsl].rearrange("g p t -> p g t"), reads=[self.b_SCTs], writes=[b_CT])
                pc, b_pc = psm.next()
                n48, b_n48 = nla48[d]
                fw.mm(pc[0:48, 0:128], [(n48[:, ti, :], tri[:])], [b_n48, b_tri], b_pc)
                cT, b_cT = cTr.next(); h2, b_h2 = h2r.next()
                fw.op(fw.act, lambda e: e.copy(out=cT[0:16, :], in_=pc[0:16, 0:128]), reads=[b_pc], pwrites=[b_cT])
                fw.op(fw.act, lambda e: e.copy(out=h2[32:48, :], in_=pc[32:48, 0:128]), reads=[b_pc], writes=[b_h2])
                fw.tt(fw.dve, cT[32:48, :], pc[32:48, 0:128], h2[32:48, :], ALU.subtract, reads=[b_pc, b_h2], pwrites=[b_cT])
                fw.dma(self.rows[ti, d, 0, :].rearrange("(h t) -> h t", h=16), cT[0:16, :], reads=[b_cT], pwrites=[self.b_rows])
                fw.dma(self.rows[ti, d, 1, :].rearrange("(h t) -> h t", h=16), cT[32:48, :], reads=[b_cT], pwrites=[self.b_rows])
                rw, b_rw = rwr.next()
                fw.dma(rw[:], self.rows[ti, d], reads=[self.b_rows], writes=[b_rw])
                yield
                CB, b_CB = CBr.next()
                for g in range(2):
                    pcb, b_pcb = psm.next()
                    fw.mm(pcb[:, 0:128], [(BT[:, g, :], CT[:, g, :])], [b_BT, b_CT], b_pcb)
                    fw.op(fw.act, lambda e: e.copy(out=CB[:, g, :], in_=pcb[:, 0:128]), reads=[b_pcb],
                          writes=[b_CB] if g == 0 else (), pwrites=() if g == 0 else [b_CB])
                yield
                Lb, b_Lb = Lr.next()
                for bk in range(4):
                    pe_, b_pe = pse.next()
                    csl = slice(512 * bk, 512 * bk + 512)
                    fw.mm(pe_[:], [(nones[0:2, :], rw[0:2, csl]), (cT[0:48, :], psel[0:48, csl]),
                                   (ident[:], n4[:].rearrange("p a b -> p (a b)"))],
                          [b_nones, b_rw, b_cT, b_psel, b_id, b_n4], b_pe)
                    fw.acti(Lb[:, 4 * bk:4 * bk + 4, :].rearrange("p a b -> p (a b)"), pe_[:], AF.Exp, reads=[b_pe],
                            writes=[b_Lb] if bk == 0 else (), pwrites=() if bk == 0 else [b_Lb])
                yield
                MT, b_MT = MTr.next()
                for g in range(2):
                    fw.tt(fw.dve, MT[:, 8 * g:8 * g + 8, :], Lb[:, 8 * g:8 * g + 8, :],
                          CB[:, g, :].unsqueeze(1).to_broadcast([128, 8, 128]), ALU.mult,
                          reads=[b_Lb, b_CB], writes=[b_MT] if g == 0 else (), pwrites=() if g == 0 else [b_MT])
                xd, b_xd = xdr.next(); xw, b_xw = xwr.next()
                x3 = x[:].rearrange("p (h e) -> p h e", h=16)
                fw.tt(fw.pool, xd[:].rearrange("p (h e) -> p h e", h=16), x3,
                      dt_[d][0][:, ti, :].unsqueeze(2).to_broadcast([128, 16, 64]), ALU.mult, reads=[b_x, dt_[d][1]], writes=[b_xd])
                fw.tt(fw.pool, xw[:].rearrange("p (h e) -> p h e", h=16), x3,
                      wd_[d][0][:, ti, :].unsqueeze(2).to_broadcast([128, 16, 64]), ALU.mult, reads=[b_x, wd_[d][1]], writes=[b_xw])
                yield
                py, b_py = pyr.next()
                for h in range(16):
                    fw.op(fw.pe, lambda e, h=h: e.matmul(py[:, h // 8, (h % 8) * 64:(h % 8) * 64 + 64], lhsT=MT[:, h, :],
                                                          rhs=xd[:, h * 64:(h + 1) * 64], start=True, stop=True, skip_group_check=True),
                          reads=[b_MT, b_xd], writes=[b_py] if h == 0 else (), pwrites=() if h == 0 else [b_py], inc=(h == 15))
                pi, b_pi = pir.next()
                for g in range(2):
                    fw.mm(pi[:, g, :], [(CT[:, g, :], Hbf[:, g * 512:(g + 1) * 512])], [b_CT, b_Hbf], b_pi, pw=(g > 0))
                yd, b_yd = ydr.next()
                for g in range(2):
                    t1, b_t1 = t1r.next()
                    fw.tt(fw.dve, t1[:].rearrange("p (h e) -> p h e", h=8), pi[:, g, :].rearrange("p (h e) -> p h e", h=8),
                          rc_[d][0][:, ti, 8 * g:8 * g + 8].unsqueeze(2).to_broadcast([128, 8, 64]), ALU.mult,
                          reads=[b_pi, rc_[d][1]], writes=[b_t1])
                    fw.tt(fw.dve, yd[:, g * 512:(g + 1) * 512], py[:, g, :], t1[:], ALU.add, reads=[b_py, b_t1],
                          writes=[b_yd] if g == 0 else (), pwrites=() if g == 0 else [b_yd])
                yield
                ph, b_ph = pir.next()
                for g in range(2):
                    fw.mm(ph[:, g, :], [(Bt[:, g * 128:(g + 1) * 128], xw[:, g * 512:(g + 1) * 512])], [b_Bt, b_xw], b_ph, pw=(g > 0))
                for g in range(2):
                    hs3 = Hst[:, g * 512:(g + 1) * 512].rearrange("p (h e) -> p h e", h=8)
                    fw.tt(fw.dve, hs3, hs3, gd_[d][0][:, ti, 8 * g:8 * g + 8].unsqueeze(2).to_broadcast([128, 8, 64]), ALU.mult,
                          reads=[b_Hst, gd_[d][1]], pwrites=[b_Hst])
                    fw.tt(fw.dve, Hst[:, g * 512:(g + 1) * 512], ph[:, g, :], Hst[:, g * 512:(g + 1) * 512], ALU.add,
                          reads=[b_ph, b_Hst], pwrites=[b_Hst])
                fw.op(fw.act, lambda e: e.copy(out=Hbf[:], in_=Hst[:]), reads=[b_Hst], writes=[b_Hbf])
                yield
                if l == 1 and ti < 2:
                    return
                if not second:
                    fw.dma(self.YFs[tsl, :], yd[:], reads=[b_yd], pwrites=[self.b_YFs])
                    return
                yf, b_yf = yfr.next(); sz, b_sz = szr.next()
                fw.dma(yf[:], self.YFs[tsl, :], reads=[self.b_YFs], writes=[b_yf])
                fw.dma(sz[:], self.SZs[tsl, :], reads=[self.b_SZs], writes=[b_sz])
                yt, b_yt = ytr.next()
                fw.tt(fw.pool, yt[:].rearrange("p (h e) -> p h e", h=16), x3, dsk[:].unsqueeze(2).to_broadcast([128, 16, 64]), ALU.mult,
                      reads=[b_x, b_dsk], writes=[b_yt])
                fw.tt(fw.pool, yt[:], yt[:], yf[:], ALU.add, reads=[b_yt, b_yf], writes=[b_yt])
                fw.tt(fw.dve, yt[:], yt[:], yd[:], ALU.add, reads=[b_yt, b_yd], writes=[b_yt])
                fw.tt(fw.dve, yt[:], yt[:], sz[:], ALU.mult, reads=[b_yt, b_sz], writes=[b_yt])
                yield
                ss, b_ss = ssr.next(); jk, b_jk = jr.next()
                fw.acti(jk[:], yt[:], AF.Square, reads=[b_yt], writes=[b_jk, b_ss], accum_out=ss[:, 0:1])
                fw.rstd(ss[:, 1:2], ss[:, 0:1], 1.0 / 1024, b_ss)
                yield
                os_, b_os = osr.next()
                fw.stt(fw.dve, os_[:], yt[:], ss[:, 1:2], nrow[:], ALU.mult, ALU.mult, reads=[b_yt, b_ss, b_nrow], writes=[b_os])
                yield
                pt, b_pt = ptr.next()
                for c in range(8):
                    fw.tr(pt[:, c, :], os_[:, c * 128:(c + 1) * 128], ident[:], [b_os, b_id], b_pt, last=(c == 7), pw=(c > 0))
                st, b_st = stg.next()
                fw.copy_any(st[:], pt[:], reads=[b_pt], writes=[b_st])
                fw.dma(self.OTs[2, :, :, tsl].rearrange("h p t -> p h t"), st[:], reads=[b_st], pwrites=[self.b_OTs])

            def lockstep(gens):
                gens = list(gens)
                while gens:
                    for g in list(gens):
                        try:
                            next(g)
                        except StopIteration:
                            gens.remove(g)

            order = (list(range(NT)), [1, 0] + list(range(NT - 1, 1, -1)))
            seen = set()
            for n in range(NT):
                gens = []
                for d in range(2):
                    ti = order[d][n]
                    gens.append(step(d, ti, ti in seen))
                    seen.add(ti)
                lockstep(gens)

    def phase_merge(self, l, s):
        fw = self.fw
        with fw.scope() as sc:
            bgT, b_bgT = sc.tile("bgT", [128, 48], F32)
            fw.dma(bgT[:], self.b_gateT[l], writes=[b_bgT])
            g2 = {}
            for j, nm in ((s, "x"), (2, "c")):
                if nm == "c" and l == 1:
                    continue
                g2[nm] = sc.tile("g2" + nm, [128, D], F32)
            with fw.scope() as s3:
                gpost, b_gpost = s3.tile("gpost", [128, D], F32)
                fw.dma(gpost[:], bcast_rows(self.g_post[l:l + 1, :]), writes=[b_gpost])
                for j, nm in ((s, "x"), (2, "c")):
                    if nm == "c" and l == 1:
                        continue
                    t, b = g2[nm]
                    fw.dma(t[:], bcast_rows(self.mods[l, j:j + 1, 2 * D:3 * D]), reads=[self.b_mods], writes=[b])
                    fw.tt(fw.dve, t[:], t[:], gpost[:], ALU.mult, reads=[b, b_gpost], writes=[b])
            groups = ([(0, 256)] if l == 0 else []) + [(256 + 512 * m, 512) for m in range(4)]
            hg, b_hg = sc.tile("hg", [128, 16, 512], BF16)
            og, b_og = sc.tile("og", [128, 3, 8, 512], BF16)
            mT, b_mT = sc.tile("mT", [128, 16, 512], BF16)
            macc, b_macc = sc.tile("macc", [128, 4, 512], F32)
            wgr = sc.ring("wg", 3, [128, 16, 512], BF16)
            wbr = sc.ring("wb", 3, [128, 8, 512], BF16)
            wor = wgr
            sgr = sc.ring("sg", 2, [128, 512], F32)
            tmr = sc.ring("tm", 2, [128, 512], F32)
            pgr = sc.ring("pg", 3, [128, 512], F32, psum=True)
            ppr = sc.ring("ppj", 3, [128, 512], F32, psum=True)
            pyr = sc.ring("pyo", 2, [128, 512], F32, psum=True)
            ysb = [sc.tile(f"ysb{i}", [128, D], F32) for i in range(4)]
            xr = sc.ring("xres", 1, [128, D], F32)
            jr = sc.ring("ej", 1, [128, D], BF16)
            ssr = sc.ring("ess", 2, [128, 2], F32)
            for gi, (g0, gs) in enumerate(groups):
                G2, b_G2 = g2["c" if g0 < CTX else "x"]
                conv = (s == 0 and gi == 0)
                fw.dma(hg[:, :, 0:gs], self.hTs[:, :, g0:g0 + gs].rearrange("k p t -> p k t"), reads=[self.b_hTs], writes=[b_hg])
                for n in range(3):
                    fw.dma(og[:, n, :, 0:gs], self.OTs[n, :, :, g0:g0 + gs].rearrange("k p t -> p k t"), reads=[self.b_OTs],
                           writes=[b_og] if n == 0 else (), pwrites=() if n == 0 else [b_og])
                for cb in range(4):
                    for n in range(3):
                        wg, b_wg = wgr.next(); wb, b_wb = wbr.next()
                        c0 = n * D + cb * 512
                        if conv:
                            fw.dma(wg[:], self.w_gate[l, :, c0:c0 + 512].rearrange("(k p) c -> p k c", p=128), writes=[b_wg], eng=fw.pool)
                            fw.dma(wb[:], self.w_branch[l, n, :, cb * 512:(cb + 1) * 512].rearrange("(k p) c -> p k c", p=128),
                                   writes=[b_wb], eng=fw.pool)
                            fw.dma(self.wg16[n * 4 + cb], wg[:].rearrange("p k c -> p (k c)"), reads=[b_wg], pwrites=[self.b_wg16])
                            fw.dma(self.wb16[n * 4 + cb], wb[:].rearrange("p k c -> p (k c)"), reads=[b_wb], pwrites=[self.b_wb16])
                        else:
                            fw.dma(wg[:].rearrange("p k c -> p (k c)"), self.wg16[n * 4 + cb], reads=[self.b_wg16], writes=[b_wg])
                            fw.dma(wb[:].rearrange("p k c -> p (k c)"), self.wb16[n * 4 + cb], reads=[self.b_wb16], writes=[b_wb])
                        for j in range(4):
                            ch = cb * 4 + j
                            pg, b_pg = pgr.next(); pp, b_pp = ppr.next()
                            fw.mm(pg[:, 0:gs], [(wg[:, k, j * 128:(j + 1) * 128], hg[:, k, 0:gs]) for k in range(16)], [b_wg, b_hg], b_pg)
                            fw.mm(pp[:, 0:gs], [(wb[:, k, j * 128:(j + 1) * 128], og[:, n, k, 0:gs]) for k in range(8)], [b_wb, b_og], b_pp)
                            sg, b_sg = sgr.next()
                            fw.acti(sg[:, 0:gs], pg[:, 0:gs], AF.Sigmoid, reads=[b_pg, b_bgT], writes=[b_sg],
                                    bias=bgT[:, n * 16 + ch:n * 16 + ch + 1])
                            if n == 0:
                                fw.tt(fw.dve, macc[:, j, 0:gs], sg[:, 0:gs], pp[:, 0:gs], ALU.mult, reads=[b_sg, b_pp],
                                      writes=[b_macc] if j == 0 else (), pwrites=() if j == 0 else [b_macc])
                            else:
                                tm, b_tm = tmr.next()
                                fw.tt(fw.dve, tm[:, 0:gs], sg[:, 0:gs], pp[:, 0:gs], ALU.mult, reads=[b_sg, b_pp], writes=[b_tm])
                                if n == 1:
                                    fw.tt(fw.pool, macc[:, j, 0:gs], macc[:, j, 0:gs], tm[:, 0:gs], ALU.add, reads=[b_tm, b_macc], pwrites=[b_macc])
                                else:
                                    first = (cb == 0 and j == 0)
                                    fw.tt(fw.pool, mT[:, ch, 0:gs], macc[:, j, 0:gs], tm[:, 0:gs], ALU.add, reads=[b_tm, b_macc],
                                          writes=[b_mT] if first else (), pwrites=() if first else [b_mT])
                nt = gs // 128
                for cb in range(4):
                    wo, b_wo = wor.next()
                    if conv:
                        fw.dma(wo[:], self.w_out[l, :, cb * 512:(cb + 1) * 512].rearrange("(k p) c -> p k c", p=128), writes=[b_wo], eng=fw.pool)
                        fw.dma(self.wo16[cb], wo[:].rearrange("p k c -> p (k c)"), reads=[b_wo], pwrites=[self.b_wo16])
                    else:
                        fw.dma(wo[:].rearrange("p k c -> p (k c)"), self.wo16[cb], reads=[self.b_wo16], writes=[b_wo])
                    for t in range(nt):
                        py, b_py = pyr.next()
                        fw.mm(py[:], [(mT[:, k, t * 128:(t + 1) * 128], wo[:, k, :]) for k in range(16)], [b_mT, b_wo], b_py)
                        yt, b_yt = ysb[t]
                        fw.copy_any(yt[:, cb * 512:(cb + 1) * 512], py[:], reads=[b_py],
                                    writes=[b_yt] if cb == 0 else (), pwrites=() if cb == 0 else [b_yt])
                for t in range(nt):
                    ti = (g0 + t * 128) // 128
                    yt, b_yt = ysb[t]
                    ss, b_ss = ssr.next(); jk, b_jk = jr.next()
                    fw.acti(jk[:], yt[:], AF.Square, reads=[b_yt], writes=[b_jk, b_ss], accum_out=ss[:, 0:1])
                    fw.rstd(ss[:, 1:2], ss[:, 0:1], 1.0 / D, b_ss)
                    xt, b_xt = xr.next()
                    fw.dma(xt[:], self.src_tile(l, s, ti), reads=[self.b_xs1] if l > 0 else [], writes=[b_xt])
                    fw.stt(fw.dve, yt[:], yt[:], ss[:, 1:2], G2[:], ALU.mult, ALU.mult, reads=[b_yt, b_ss, b_G2], writes=[b_yt])
                    fw.tt(fw.pool, xt[:], xt[:], yt[:], ALU.add, reads=[b_xt, b_yt], writes=[b_xt])
                    if l == 0:
                        fw.dma(self.xs1[s, ti * 128:(ti + 1) * 128, :], xt[:], reads=[b_xt], pwrites=[self.b_xs1])
                    else:
                        fw.dma(self.out[s, (ti - 2) * 128:(ti - 1) * 128, :], xt[:], reads=[b_xt], pwrites=[self.b_out])


def build(plan=None, debug=()):
    p = Prog(debug=debug)
    if plan is None:
        plan = [(l, s) for l in range(2) for s in range(2)]
    p.plan = plan
    p.phase_mod()
    stages = getattr(build, "stages", "PBCDE")
    for (l, s) in plan:
        if "P" in stages:
            p.phase_proj(l, s)
        if "B" in stages:
            p.phase_att(l, s)
        if "C" in stages:
            p.phase_mlstm(l, s)
        if "D" in stages:
            p.phase_ssd(l, s)
        if "E" in stages:
            p.phase_merge(l, s)
    p.fw.finish()
    return p


def fmaj(v, chunks):
    return np.ascontiguousarray(np.asarray(v, np.float32).reshape(chunks, 128).T)


def shared_inputs(inp):
    cosT, sinT = make_rope()
    sh = {
        "w_mod": inp["w_mod"], "w_in": inp["w_in"], "w_gate": inp["w_gate"], "w_branch": inp["w_branch"],
        "w_out": inp["w_out"], "b_mod": inp["b_mod"], "g_pre": inp["g_pre"], "g_post": inp["g_post"],
        "b_gateT": np.stack([fmaj(inp["b_gate"][l], 48) for l in range(2)]),
        "att_sink": inp["att_sink"], "ml_gb": np.asarray(inp["ml_gate_bias"], np.float32).reshape(2, 16),
        "ml_norm": inp["ml_norm"],
        "conv_wT": np.stack([np.ascontiguousarray(np.asarray(inp["ssm_conv_w"][l]).T.reshape(12, 128, 5).transpose(1, 0, 2))
                             for l in range(2)]),
        "conv_bT": np.stack([fmaj(inp["ssm_conv_b"][l], 12) for l in range(2)]),
        "dt_bias": np.asarray(inp["ssm_dt_bias"], np.float32).reshape(2, 32),
        "a_log": np.asarray(inp["ssm_a_log"], np.float32).reshape(2, 32),
        "ssm_d": inp["ssm_d"], "ssm_norm": inp["ssm_norm"],
        "consts": make_consts(), "cosT": cosT, "sinT": sinT,
    }
    return {k: np.ascontiguousarray(np.asarray(v, np.float32)) for k, v in sh.items()}


def core_inputs(inp, sh, core):
    b0 = 2 * core
    cst = np.stack([fmaj(inp["c"][b0], 16), fmaj(inp["c"][b0 + 1], 16), fmaj(inp["c_ctx"], 16)], axis=-1)
    m = dict(sh)
    m["xin"] = np.ascontiguousarray(np.asarray(inp["x"][b0:b0 + 2], np.float32))
    m["ctxin"] = np.ascontiguousarray(np.asarray(inp["ctx"][b0:b0 + 2], np.float32))
    m["cst"] = np.ascontiguousarray(cst.astype(np.float32))
    return m


def kernel(**inputs):
    p = build()
    sh = shared_inputs(inputs)
    in_maps = [core_inputs(inputs, sh, c) for c in range(NCORES)]
    res = run_bass_kernel_spmd(p.nc, in_maps, core_ids=list(range(NCORES)))
    return np.concatenate([np.asarray(r["out"], np.float32) for r in res.results], axis=0)
```
